# Optimizing a Trainium2 kernel written in Bass

```python
import math
import jax, jax.numpy as jnp
from jax import lax
import numpy as np

D_MODEL = 2048
BATCH = 1
SEQ = 16384
DEPTH = 4

N_META = 16
MIX_WIDTH = D_MODEL
CONV_HEADS = 8
CONV_CH = MIX_WIDTH // 2
CONV_K = 31
POOL_WINDOWS = (2, 4, 8, 16)
POOL_GROUPS = len(POOL_WINDOWS)
POOL_CH = MIX_WIDTH - CONV_CH
POOL_GROUP_CH = POOL_CH // POOL_GROUPS
IN_COLS = 2 * CONV_CH + POOL_CH
N_EXPERT_GROUPS = 4
EXPERTS_PER_GROUP = 8
N_EXPERTS = N_EXPERT_GROUPS * EXPERTS_PER_GROUP
TOP_K = 2
EXPERT_HIDDEN = D_MODEL // 4
EXPERT_BLOCK = 128
DEEPNORM_ALPHA = (2.0 * DEPTH) ** 0.25
DEEPNORM_BETA = (8.0 * DEPTH) ** -0.25
LN_EPS = 1e-5

kernel_name = "hymba_conformer_pool_hmoe_deepnorm"


def layer_norm(x, g, b):
    xf = x.astype(jnp.float32)
    mu = jnp.mean(xf, axis=-1, keepdims=True)
    xc = xf - mu
    var = jnp.mean(xc * xc, axis=-1, keepdims=True)
    y = xc * lax.rsqrt(var + LN_EPS)
    return (y * g.astype(jnp.float32) + b.astype(jnp.float32)).astype(x.dtype)


def multiscale_pool(u, pool_w, pool_b, pool_scale):
    B, L, C = u.shape
    csp = jnp.pad(lax.cumsum(u.astype(jnp.float32), axis=1), ((0, 0), (1, 0), (0, 0)))
    t = jnp.arange(L)
    means = []
    for g, w in enumerate(POOL_WINDOWS):
        sl = slice(g * POOL_GROUP_CH, (g + 1) * POOL_GROUP_CH)
        lo = jnp.maximum(t + 1 - w, 0)
        cnt = (t + 1 - lo).astype(jnp.float32)[None, :, None]
        means.append((csp[:, 1:, sl] - csp[:, lo, sl]) / cnt)
    pooled = jnp.stack(means, axis=2)
    ug = u.reshape(B, L, POOL_GROUPS, POOL_GROUP_CH)
    d = (pooled - ug.astype(jnp.float32)).astype(u.dtype)
    y = jnp.einsum('blgc,gce->blge', d, pool_w) + pool_b
    return y.reshape(B, L, C) * pool_scale


def hybrid_mixer(h, w_in, b_in, conv_w, conv_b, conv_ln_g, conv_ln_b, pool_w, pool_b, pool_scale, w_out, b_out):
    proj = jnp.einsum('bld,dc->blc', h, w_in) + b_in
    a = proj[..., :CONV_CH]
    gate = proj[..., CONV_CH:2 * CONV_CH]
    u = proj[..., 2 * CONV_CH:]
    v = a * jax.nn.sigmoid(gate)
    v = lax.conv_general_dilated(v, conv_w, window_strides=(1,), padding=[(CONV_K - 1, 0)],
                                 dimension_numbers=('NWC', 'WIO', 'NWC'),
                                 feature_group_count=CONV_CH) + conv_b
    v = jax.nn.silu(layer_norm(v, conv_ln_g, conv_ln_b))
    p = multiscale_pool(u, pool_w, pool_b, pool_scale)
    y = jnp.concatenate([v, p], axis=-1)
    return jnp.einsum('blc,cd->bld', y, w_out) + b_out


def hierarchical_moe(h, rg_w, rg_b, re_w, re_b, w_gate, w_up, w_down):
    B, L, D = h.shape
    T = B * L
    xf = h.reshape(T, D)
    g_prob = jax.nn.softmax((xf @ rg_w + rg_b).astype(jnp.float32), axis=-1)
    g_idx = jnp.argmax(g_prob, axis=-1)
    g_p = jnp.take_along_axis(g_prob, g_idx[:, None], axis=1)[:, 0]
    e_logits = (xf @ re_w + re_b).astype(jnp.float32).reshape(T, N_EXPERT_GROUPS, EXPERTS_PER_GROUP)
    e_logits = jnp.take_along_axis(e_logits, g_idx[:, None, None], axis=1)[:, 0]
    e_prob = jax.nn.softmax(e_logits, axis=-1)
    top_p, top_i = lax.top_k(e_prob, TOP_K)
    top_p = top_p / jnp.sum(top_p, axis=-1, keepdims=True)
    gates = g_p[:, None] * top_p
    expert = (g_idx[:, None] * EXPERTS_PER_GROUP + top_i).astype(jnp.int32)
    n_assign = T * TOP_K
    e_flat = expert.reshape(-1)
    gate_flat = gates.reshape(-1)
    tok_flat = jnp.repeat(jnp.arange(T, dtype=jnp.int32), TOP_K)
    counts = jnp.bincount(e_flat, length=N_EXPERTS)
    padded = (counts + EXPERT_BLOCK - 1) // EXPERT_BLOCK * EXPERT_BLOCK
    start = jnp.cumsum(counts) - counts
    pend = jnp.cumsum(padded)
    pstart = pend - padded
    order = jnp.argsort(e_flat, stable=True)
    se = e_flat[order]
    dest = pstart[se] + jnp.arange(n_assign) - start[se]
    n_blocks = -(-(n_assign + N_EXPERTS * (EXPERT_BLOCK - 1)) // EXPERT_BLOCK)
    n_slots = n_blocks * EXPERT_BLOCK
    slot_tok = jnp.full((n_slots,), T, jnp.int32).at[dest].set(tok_flat[order])
    slot_gate = jnp.zeros((n_slots,), jnp.float32).at[dest].set(gate_flat[order])
    block_expert = jnp.minimum(jnp.searchsorted(pend, jnp.arange(n_blocks) * EXPERT_BLOCK, side='right'),
                               N_EXPERTS - 1)
    x_pad = jnp.concatenate([xf, jnp.zeros((1, D), xf.dtype)], axis=0)
    x_blocks = x_pad[slot_tok].reshape(n_blocks, EXPERT_BLOCK, D)

    def expert_block(args):
        xb, e = args
        hb = jax.nn.silu(xb @ w_gate[e]) * (xb @ w_up[e])
        return hb @ w_down[e]

    y = lax.map(expert_block, (x_blocks, block_expert)).reshape(n_slots, D)
    y = y * slot_gate[:, None].astype(y.dtype)
    out = jnp.zeros((T + 1, D), y.dtype).at[slot_tok].add(y)[:T]
    return out.reshape(B, L, D)


def setup_inputs(seed: int = 0) -> dict:
    key = jax.random.key(seed)
    ks = jax.random.split(key, 32)
    n = lambda k, s: jax.random.normal(k, s, jnp.float32)
    D = D_MODEL
    return {
        "x": n(ks[0], (BATCH, SEQ, D)),
        "meta_tokens": n(ks[1], (N_META, D)),
        "ln_in_g": 1.0 + 0.02 * n(ks[2], (D,)),
        "ln_in_b": 0.02 * n(ks[3], (D,)),
        "w_in": n(ks[4], (DEPTH, D, IN_COLS)) * D ** -0.5,
        "b_in": 0.02 * n(ks[5], (DEPTH, IN_COLS)),
        "conv_w": n(ks[6], (DEPTH, CONV_K, 1, CONV_CH)) * CONV_K ** -0.5,
        "conv_b": 0.02 * n(ks[7], (DEPTH, CONV_CH)),
        "conv_ln_g": 1.0 + 0.02 * n(ks[8], (DEPTH, CONV_CH)),
        "conv_ln_b": 0.02 * n(ks[9], (DEPTH, CONV_CH)),
        "pool_w": n(ks[10], (DEPTH, POOL_GROUPS, POOL_GROUP_CH, POOL_GROUP_CH)) * POOL_GROUP_CH ** -0.5,
        "pool_b": 0.02 * n(ks[11], (DEPTH, POOL_GROUPS, POOL_GROUP_CH)),
        "pool_scale": 1.0 + 0.02 * n(ks[12], (DEPTH, POOL_CH)),
        "w_out": n(ks[13], (DEPTH, MIX_WIDTH, D)) * (MIX_WIDTH ** -0.5 * DEEPNORM_BETA),
        "b_out": 0.02 * n(ks[14], (DEPTH, D)),
        "ln_mix_g": 1.0 + 0.02 * n(ks[15], (DEPTH, D)),
        "ln_mix_b": 0.02 * n(ks[16], (DEPTH, D)),
        "router_group_w": n(ks[17], (DEPTH, D, N_EXPERT_GROUPS)) * D ** -0.5,
        "router_group_b": 0.01 * n(ks[18], (DEPTH, N_EXPERT_GROUPS)),
        "router_expert_w": n(ks[19], (DEPTH, D, N_EXPERTS)) * D ** -0.5,
        "router_expert_b": 0.01 * n(ks[20], (DEPTH, N_EXPERTS)),
        "w_gate": n(ks[21], (DEPTH, N_EXPERTS, D, EXPERT_HIDDEN)) * D ** -0.5,
        "w_up": n(ks[22], (DEPTH, N_EXPERTS, D, EXPERT_HIDDEN)) * D ** -0.5,
        "w_down": n(ks[23], (DEPTH, N_EXPERTS, EXPERT_HIDDEN, D)) * (EXPERT_HIDDEN ** -0.5 * DEEPNORM_BETA),
        "ln_ffn_g": 1.0 + 0.02 * n(ks[24], (DEPTH, D)),
        "ln_ffn_b": 0.02 * n(ks[25], (DEPTH, D)),
    }


def reference(x, meta_tokens, ln_in_g, ln_in_b, w_in, b_in, conv_w, conv_b, conv_ln_g, conv_ln_b,
              pool_w, pool_b, pool_scale, w_out, b_out, ln_mix_g, ln_mix_b,
              router_group_w, router_group_b, router_expert_w, router_expert_b,
              w_gate, w_up, w_down, ln_ffn_g, ln_ffn_b):
    B = x.shape[0]
    meta = jnp.broadcast_to(meta_tokens.astype(x.dtype)[None], (B, N_META, x.shape[-1]))
    h = jnp.concatenate([meta, x], axis=1)
    h = layer_norm(h, ln_in_g, ln_in_b)
    for i in range(DEPTH):
        mix = hybrid_mixer(h, w_in[i], b_in[i], conv_w[i], conv_b[i], conv_ln_g[i], conv_ln_b[i],
                           pool_w[i], pool_b[i], pool_scale[i], w_out[i], b_out[i])
        h = layer_norm(DEEPNORM_ALPHA * h + mix, ln_mix_g[i], ln_mix_b[i])
        ffn = hierarchical_moe(h, router_group_w[i], router_group_b[i], router_expert_w[i], router_expert_b[i],
                               w_gate[i], w_up[i], w_down[i])
        h = layer_norm(DEEPNORM_ALPHA * h + ffn, ln_ffn_g[i], ln_ffn_b[i])
    return h[:, N_META:]
```

```python
import numpy as np
import ml_dtypes
import concourse.bass as bass
import concourse.mybir as mybir
from concourse.bass_utils import run_bass_kernel_spmd

F32 = mybir.dt.float32
BF16 = mybir.dt.bfloat16
I32 = mybir.dt.int32
AF = mybir.ActivationFunctionType
ALU = mybir.AluOpType
AX = mybir.AxisListType

D = 2048
SEQ = 16384
DEPTH = 4
NMETA = 16
NCORES = 8
TOK_OUT = SEQ // NCORES
HALO = 128
T = TOK_OUT + HALO
NT = T // 128
KD = D // 128
CONV_CH = 1024
CONV_K = 31
NEXP = 32
EH = 512
CAP = 256
NSLOT = NEXP * CAP
TRASH = NSLOT
ALPHA = float((2.0 * DEPTH) ** 0.25)
EPS = 1e-5
POOL_W = (2, 4, 8, 16)
TB = [(0, 512), (512, 512), (1024, 512), (1536, 512), (2048, 128)]


PHASE_MARKS = []


def merge(dst, src):
    for k, (s, v) in src.items():
        if k not in dst or dst[k][1] < v:
            dst[k] = (s, v)


class Buf:
    def __init__(self, name=""):
        self.name = name
        self.w = {}
        self.r = {}


class KB:
    def __init__(self, nc):
        self.nc = nc
        self.eng = {"pe": nc.tensor, "act": nc.scalar, "dve": nc.vector, "pool": nc.gpsimd, "sp": nc.sync}
        self.sem = {}
        self.cnt = {}
        self.waited = {e: {} for e in self.eng}
        self.emitted = {e: 0 for e in self.eng}
        self._cms = []
        for e in ("pe", "act", "dve", "pool"):
            cm = nc.semaphore("p_" + e)
            self.sem[e] = cm.__enter__()
            self._cms.append(cm)
            self.cnt[e] = 0
        self.ring = {}
        self.ringpos = {}
        for q, n in (("sp", 20), ("pool", 28), ("act", 8)):
            lst = []
            for i in range(n):
                cm = nc.semaphore("d_%s%d" % (q, i))
                lst.append([cm.__enter__(), 0])
                self._cms.append(cm)
            self.ring[q] = lst
            self.ringpos[q] = 0

    def close(self):
        for cm in reversed(self._cms):
            cm.__exit__(None, None, None)

    def _wait(self, e, deps):
        w = self.waited[e]
        for k, (s, v) in deps.items():
            if v > 0 and w.get(k, 0) < v:
                self.eng[e].wait_ge(s, v)
                self.emitted[e] += 1
                w[k] = v

    def _deps(self, e, reads, writes, accw):
        deps = {}
        for b in reads:
            merge(deps, b.w)
        for b in writes:
            merge(deps, b.w)
            merge(deps, b.r)
        for b in accw:
            merge(deps, b.r)
        if e == "pe":
            deps.pop("pe", None)
        return deps

    def _post(self, tok, reads, writes, accw):
        for b in reads:
            merge(b.r, tok)
        for b in writes:
            b.w = dict(tok)
            b.r = {}
        for b in accw:
            merge(b.w, tok)

    def op(self, e, fn, reads=(), writes=(), accw=(), inc=True):
        self._wait(e, self._deps(e, reads, writes, accw))
        inst = fn()
        self.emitted[e] += 1
        if inc:
            self.cnt[e] += 1
            inst.then_inc(self.sem[e], 1)
            tok = {e: (self.sem[e], self.cnt[e])}
        else:
            tok = {e: (self.sem[e], self.cnt[e] + 1)}
        self._post(tok, reads, writes, accw)
        return tok

    def dma(self, q, fn, reads=(), writes=(), accw=()):
        self._wait(q, self._deps(q, reads, writes, accw))
        lst = self.ring[q]
        i = self.ringpos[q]
        self.ringpos[q] = (i + 1) % len(lst)
        ent = lst[i]
        key = "d_%s%d" % (q, i)
        self._wait(q, {key: (ent[0], ent[1])})
        inst = fn()
        self.emitted[q] += 1
        ent[1] += 16
        inst.then_inc(ent[0], 16)
        tok = {key: (ent[0], ent[1])}
        self._post(tok, reads, writes, accw)
        return tok

    def barrier(self):
        PHASE_MARKS.append(dict(self.emitted))
        deps = {}
        for e in ("pe", "act", "dve", "pool"):
            deps[e] = (self.sem[e], self.cnt[e])
        for q, lst in self.ring.items():
            for i, ent in enumerate(lst):
                deps["d_%s%d" % (q, i)] = (ent[0], ent[1])
        for e in ("pe", "act", "dve", "pool", "sp"):
            self._wait(e, deps)

    def wait_all(self, e, bufs):
        deps = {}
        for b in bufs:
            merge(deps, b.w)
            merge(deps, b.r)
        self._wait(e, deps)


class Arena:
    def __init__(self, ap_f32, nbytes):
        self.ap = ap_f32
        self.nbytes = nbytes
        self.off = 0

    def mark(self):
        return self.off

    def release(self, m):
        self.off = m

    def alloc(self, dtype, free_shape):
        esz = 4 if dtype in (F32, I32) else 2
        n = int(np.prod(free_shape))
        nb = (n * esz + 31) // 32 * 32
        assert self.off + nb <= self.nbytes, ("arena overflow", self.off, nb, self.nbytes)
        v = self.ap[:, self.off // 4:(self.off + nb) // 4]
        if dtype != F32:
            v = v.bitcast(dtype)
        v = v[:, 0:n]
        self.off += nb
        if len(free_shape) == 2:
            v = v.rearrange("p (a b) -> p a b", a=free_shape[0])
        elif len(free_shape) == 3:
            v = v.rearrange("p (a b c) -> p a b c", a=free_shape[0], b=free_shape[1])
        return v


def build_program(depth=DEPTH, stop_after=None, debug=False):
    nc = bass.Bass("TRN2", target_bir_lowering=False)
    L = DEPTH

    def din(name, shape, dt=F32):
        return nc.dram_tensor(name, list(shape), dt, kind="ExternalInput").ap()

    x_in = din("x", [T, D])
    tmask_in = din("tmask", [128, 128])
    invcnt_in = din("invcnt", [128, 4, 128])
    tokv_in = din("tokv", [128, 4])
    consts_in = din("consts", [128, 3, 128])
    ecvec_in = din("ecvec", [128, NEXP])
    lnin_in = din("lnin", [2, 128, D])
    vecs_in = din("vecs", [L, 5, 128, D])
    w_in_d = din("w_in", [L, D, 3072])
    w_out_d = din("w_out", [L, D, D])
    pool_w_d = din("pool_w", [L, 4, 256, 256])
    bin_d = din("b_in_t", [L, 128, 24])
    convw_d = din("conv_w_t", [L, 128, 8, CONV_K])
    cvec_d = din("cvec", [L, 128, 5, 8])
    wr_d = din("wr", [L, D, 36])
    br_d = din("br", [L, 128, 36])
    wg_d = din("w_gate", [L, NEXP, D, EH])
    wu_d = din("w_up", [L, NEXP, D, EH])
    wd_d = din("w_down", [L, NEXP, EH, D])
    out_d = nc.dram_tensor("out", [TOK_OUT, D], F32, kind="ExternalOutput").ap()
    skind = "ExternalOutput" if debug else "Internal"
    hres = nc.dram_tensor("hres", [T, D], F32, kind=skind).ap()
    cT = nc.dram_tensor("cT", [8, 128, T], F32, kind=skind).ap()
    dbg_y = nc.dram_tensor("dbg_y", [3, 128, 8, T], BF16, kind=skind).ap() if debug else None
    dbg_r = nc.dram_tensor("dbg_r", [2, 128, NT, 2], F32, kind=skind).ap() if debug else None
    xs = nc.dram_tensor("xs", [NSLOT + 128, D], BF16, kind="Internal").ap()
    ys = nc.dram_tensor("ys", [NSLOT + 128, D], F32, kind="Internal").ap()
    B_hres = [Buf("hres%d" % i) for i in range(NT)]
    B_cT = [Buf("cT%d" % j) for j in range(8)]
    B_xs = Buf("xs")
    B_ys = Buf("ys")

    ARENA_BYTES = 206 * 1024
    arena_cm = nc.sbuf_tensor("arena", [128, ARENA_BYTES // 4], F32)
    arena_t = arena_cm.__enter__()
    ar = Arena(arena_t[:, :], ARENA_BYTES)
    ps_cms = [nc.psum_tensor("ps%d" % i, [128, 512], F32) for i in range(8)]
    PS = [cm.__enter__() for cm in ps_cms]
    B_PS = [Buf("ps%d" % i) for i in range(8)]
    kb = KB(nc)

    Rf = [ar.alloc(F32, [4 * T]) for _ in range(3)]
    R = [r.bitcast(BF16).rearrange("p (a b) -> p a b", a=8) for r in Rf]
    B_R = [Buf("R%d" % i) for i in range(3)]
    consts = ar.alloc(F32, [3, 128])
    ident, tri, ones = consts[:, 0, :], consts[:, 1, :], consts[:, 2, :]
    ecvec = ar.alloc(F32, [NEXP])
    tmask = ar.alloc(F32, [128])
    invcnt = ar.alloc(F32, [4, 128])
    tokv = ar.alloc(F32, [4])
    dest_f = ar.alloc(F32, [NT, 2])
    dest_i = ar.alloc(I32, [NT, 2])
    gates = ar.alloc(F32, [NT, 2])
    base = ar.alloc(F32, [NEXP])
    B_const = Buf("const")
    B_route = [Buf("route%d" % i) for i in range(NT)]
    B_base = Buf("base")
    kb.dma("sp", lambda: nc.sync.dma_start(out=consts, in_=consts_in), writes=[B_const])
    kb.dma("sp", lambda: nc.sync.dma_start(out=ecvec, in_=ecvec_in), accw=[B_const])
    kb.dma("sp", lambda: nc.sync.dma_start(out=tmask, in_=tmask_in), accw=[B_const])
    kb.dma("sp", lambda: nc.sync.dma_start(out=invcnt, in_=invcnt_in), accw=[B_const])
    kb.dma("sp", lambda: nc.sync.dma_start(out=tokv, in_=tokv_in), accw=[B_const])
    persist_mark = ar.mark()

    def hT_chunk(regions, k):
        return R[regions[k // 8]][:, k % 8, :]

    def ln_tile(z, B_z, gv, bv, B_vec, small, B_small):
        st = small[:, 0:24].rearrange("p (a b) -> p a b", a=4)
        mv = small[:, 24:26]
        for c in range(4):
            kb.op("dve", lambda c=c: nc.vector.bn_stats(out=st[:, c, :], in_=z[:, c * 512:(c + 1) * 512]),
                  reads=[B_z], accw=[B_small] if c else (), writes=() if c else [B_small])
        kb.op("dve", lambda: nc.vector.bn_aggr(out=mv, in_=st), reads=[B_small], accw=[B_small])
        kb.op("act", lambda: nc.scalar.activation(out=small[:, 26:27], in_=small[:, 25:26], func=AF.Ln, bias=EPS, scale=1.0),
              reads=[B_small], accw=[B_small])
        kb.op("act", lambda: nc.scalar.activation(out=small[:, 27:28], in_=small[:, 26:27], func=AF.Exp, scale=-0.5),
              reads=[B_small], accw=[B_small])
        kb.op("dve", lambda: nc.vector.tensor_scalar(out=z, in0=z, scalar1=small[:, 24:25], scalar2=small[:, 27:28],
                                                     op0=ALU.subtract, op1=ALU.mult), reads=[B_small, B_z], writes=[B_z])
        kb.op("pool", lambda: nc.gpsimd.tensor_tensor(out=z, in0=z, in1=gv, op=ALU.mult), reads=[B_vec, B_z], writes=[B_z])
        kb.op("pool", lambda: nc.gpsimd.tensor_tensor(out=z, in0=z, in1=bv, op=ALU.add), reads=[B_vec, B_z], writes=[B_z])

    def transposes_to(z, B_z, dst_fn, B_dst_list, banks, evac_engs):
        for g in range(4):
            bk = banks[g % len(banks)]
            for q in range(4):
                k = 4 * g + q
                kb.op("pe", lambda k=k, q=q, bk=bk: nc.tensor.transpose(out=PS[bk][:, q * 128:(q + 1) * 128],
                                                                         in_=z[:, k * 128:(k + 1) * 128], identity=ident),
                      reads=[B_z, B_const], writes=[B_PS[bk]] if q == 0 else (), accw=() if q == 0 else [B_PS[bk]])
            e = evac_engs[g % len(evac_engs)]
            src = PS[bk][:, :].rearrange("p (a b) -> p a b", a=4)
            if e == "act":
                kb.op("act", lambda g=g, src=src: nc.scalar.copy(out=dst_fn(g), in_=src), reads=[B_PS[bk]], accw=[B_dst_list[g]])
            else:
                kb.op("dve", lambda g=g, src=src: nc.vector.tensor_copy(out=dst_fn(g), in_=src), reads=[B_PS[bk]], accw=[B_dst_list[g]])

    def phase_input_ln():
        m = ar.mark()
        zt0 = ar.alloc(F32, [D])
        B_zt0 = Buf("zt0")
        kb.op("dve", lambda: nc.vector.memset(zt0, 0.0), writes=[B_zt0])
        kb.dma("sp", lambda: nc.sync.dma_start(out=ys[NSLOT:NSLOT + 128, :], in_=zt0), reads=[B_zt0], accw=[B_ys])
        vec = ar.alloc(F32, [2, D])
        B_vec = Buf("lnin")
        kb.dma("sp", lambda: nc.sync.dma_start(out=vec, in_=lnin_in.rearrange("a p d -> p a d")), writes=[B_vec])
        xt = [ar.alloc(F32, [D]) for _ in range(2)]
        B_xt = [Buf("xt0"), Buf("xt1")]
        small = [ar.alloc(F32, [40]) for _ in range(2)]
        B_small = [Buf("sm0"), Buf("sm1")]
        for b in (B_R[0], B_R[1]):
            pass
        def load_x(i):
            kb.dma("sp", lambda: nc.sync.dma_start(out=xt[i % 2], in_=x_in[i * 128:(i + 1) * 128, :]), writes=[B_xt[i % 2]])
        load_x(0)
        for i in range(NT):
            s = i % 2
            if i + 1 < NT:
                load_x(i + 1)
            ln_tile(xt[s], B_xt[s], vec[:, 0, :], vec[:, 1, :], B_vec, small[s], B_small[s])
            kb.dma("sp", lambda i=i, s=s: nc.sync.dma_start(out=hres[i * 128:(i + 1) * 128, :], in_=xt[s]), reads=[B_xt[s]], writes=[B_hres[i]])

            def dst(g, i=i):
                k0 = 4 * g
                return R[k0 // 8][:, (k0 % 8):(k0 % 8) + 4, i * 128:(i + 1) * 128]
            transposes_to(xt[s], B_xt[s], dst, [B_R[0], B_R[0], B_R[1], B_R[1]], banks=[0, 1, 2, 3], evac_engs=["act", "dve"])
        kb.barrier()
        ar.release(m)

    def phase_m1(l):
        m = ar.mark()
        bint = ar.alloc(F32, [24])
        convw = ar.alloc(F32, [8, CONV_K])
        cvec = ar.alloc(F32, [5, 8])
        B_sv = Buf("smallvecs")
        kb.dma("sp", lambda: nc.sync.dma_start(out=bint, in_=bin_d[l]), writes=[B_sv])
        kb.dma("sp", lambda: nc.sync.dma_start(out=convw, in_=convw_d[l]), accw=[B_sv])
        kb.dma("sp", lambda: nc.sync.dma_start(out=cvec, in_=cvec_d[l]), accw=[B_sv])
        NWB = 4
        wch = [ar.alloc(BF16, [KD, 128]) for _ in range(NWB)]
        B_wch = [Buf("wch%d" % i) for i in range(NWB)]
        wcount = [0]
        w_in_v = w_in_d[l].rearrange("(k p) c -> p k c", p=128)

        def load_w(col_chunk):
            i = wcount[0] % NWB
            wcount[0] += 1
            kb.dma("pool", lambda: nc.gpsimd.dma_start(out=wch[i], in_=w_in_v[:, :, col_chunk * 128:(col_chunk + 1) * 128]),
                   writes=[B_wch[i]])
            return i

        def proj(wi, bank, t0, n):
            for k in range(KD):
                kb.op("pe", lambda k=k: nc.tensor.matmul(out=PS[bank][:, 0:n], lhsT=wch[wi][:, k, :],
                                                         rhs=hT_chunk((0, 1), k)[:, t0:t0 + n], start=(k == 0), stop=(k == KD - 1)),
                      reads=[B_wch[wi], B_R[0], B_R[1]], writes=[B_PS[bank]] if k == 0 else (),
                      accw=() if k == 0 else [B_PS[bank]], inc=(k == KD - 1))

        pm = ar.mark()
        PADU = 16
        u = ar.alloc(F32, [2, PADU + T])
        sa = ar.alloc(F32, [2, PADU + T])
        sb = ar.alloc(F32, [2, PADU + T])
        dbf = ar.alloc(BF16, [2, T])
        pw = ar.alloc(BF16, [2, 256])
        B_u, B_sa, B_sb, B_dbf, B_pw = Buf("u"), Buf("sa"), Buf("sb"), Buf("dbf"), Buf("pw")
        kb.op("dve", lambda: nc.vector.memset(u[:, :, 0:PADU], 0.0), writes=[B_u])
        kb.op("dve", lambda: nc.vector.memset(sa[:, :, 0:PADU], 0.0), writes=[B_sa])
        kb.op("dve", lambda: nc.vector.memset(sb[:, :, 0:PADU], 0.0), writes=[B_sb])
        pre = [load_w(16), load_w(17)]
        for g in range(4):
            wis = pre
            for cc in range(2):
                for bi, (t0, n) in enumerate(TB):
                    bank = (cc * len(TB) + bi) % 2
                    proj(wis[cc], bank, t0, n)
                    kb.op("act", lambda cc=cc, t0=t0, n=n, bank=bank, g=g: nc.scalar.activation(
                        out=u[:, cc, PADU + t0:PADU + t0 + n], in_=PS[bank][:, 0:n], func=AF.Identity,
                        bias=bint[:, 16 + 2 * g + cc:16 + 2 * g + cc + 1], scale=1.0),
                        reads=[B_PS[bank], B_sv], accw=[B_u])
            if g < 3:
                pre = [load_w(16 + 2 * (g + 1)), load_w(16 + 2 * (g + 1) + 1)]
            kb.dma("pool", lambda g=g: nc.gpsimd.dma_start(out=pw, in_=pool_w_d[l, g].rearrange("(a p) c -> p a c", p=128)),
                   writes=[B_pw])
            kb.op("dve", lambda: nc.vector.tensor_tensor(out=u[:, :, PADU:PADU + 128], in0=u[:, :, PADU:PADU + 128],
                                                         in1=tmask.unsqueeze(1).broadcast_to([128, 2, 128]), op=ALU.mult),
                  reads=[B_const, B_u], writes=[B_u])
            src, B_src = u, B_u
            bufs = [(sa, B_sa), (sb, B_sb)]
            step = 1
            nstep = g + 1
            for si in range(nstep):
                dstb, B_dstb = bufs[si % 2]
                kb.op("dve", lambda src=src, dstb=dstb, step=step: nc.vector.tensor_tensor(
                    out=dstb[:, :, PADU:PADU + T], in0=src[:, :, PADU:PADU + T], in1=src[:, :, PADU - step:PADU - step + T], op=ALU.add),
                    reads=[B_src], accw=[B_dstb])
                src, B_src = dstb, B_dstb
                step *= 2
            wdw = POOL_W[g]
            kb.op("dve", lambda src=src, g=g: nc.vector.tensor_tensor(
                out=src[:, :, PADU:PADU + 128], in0=src[:, :, PADU:PADU + 128],
                in1=invcnt[:, g, :].unsqueeze(1).broadcast_to([128, 2, 128]), op=ALU.mult), reads=[B_const, B_src], writes=[B_src])
            kb.op("dve", lambda src=src: nc.vector.tensor_tensor(
                out=dbf[:, :, 0:128], in0=src[:, :, PADU:PADU + 128], in1=u[:, :, PADU:PADU + 128], op=ALU.subtract),
                reads=[B_src, B_u], writes=[B_dbf])
            kb.op("dve", lambda src=src, wdw=wdw: nc.vector.scalar_tensor_tensor(
                out=dbf[:, :, 128:T], in0=src[:, :, PADU + 128:PADU + T], scalar=1.0 / wdw, in1=u[:, :, PADU + 128:PADU + T],
                op0=ALU.mult, op1=ALU.subtract), reads=[B_src, B_u], accw=[B_dbf])
            for b in range(2):
                for bi, (t0, n) in enumerate(TB):
                    bank = 2 + (b * len(TB) + bi) % 2
                    for a in range(2):
                        kb.op("pe", lambda a=a, b=b, t0=t0, n=n, bank=bank: nc.tensor.matmul(
                            out=PS[bank][:, 0:n], lhsT=pw[:, a, b * 128:(b + 1) * 128], rhs=dbf[:, a, t0:t0 + n],
                            start=(a == 0), stop=(a == 1)), reads=[B_pw, B_dbf],
                            writes=[B_PS[bank]] if a == 0 else (), accw=() if a == 0 else [B_PS[bank]], inc=(a == 1))
                    ch = 2 * g + b
                    kb.op("dve", lambda ch=ch, t0=t0, n=n, bank=bank: nc.vector.tensor_scalar(
                        out=R[2][:, ch, t0:t0 + n], in0=PS[bank][:, 0:n], scalar1=cvec[:, 3, ch:ch + 1], scalar2=cvec[:, 4, ch:ch + 1],
                        op0=ALU.add, op1=ALU.mult), reads=[B_PS[bank], B_sv], accw=[B_R[2]])
        kb.barrier()
        ar.release(pm)
        PADV = 32
        PE_TAPS = 18
        vv = [ar.alloc(F32, [PADV + T]) for _ in range(2)]
        vbf = [ar.alloc(BF16, [PADV + T]) for _ in range(2)]
        dg = [ar.alloc(BF16, [PE_TAPS, 128]) for _ in range(2)]
        sig = [ar.alloc(F32, [512]) for _ in range(2)]
        cacc = [ar.alloc(F32, [T]) for _ in range(2)]
        sq = ar.alloc(F32, [T])
        s1 = ar.alloc(F32, [T])
        s2 = ar.alloc(F32, [T])
        B_vv, B_vbf, B_dg = [Buf("v0"), Buf("v1")], [Buf("vbf0"), Buf("vbf1")], [Buf("dg0"), Buf("dg1")]
        B_sig, B_cacc, B_sq, B_s1, B_s2 = [Buf("sig0"), Buf("sig1")], [Buf("c0"), Buf("c1")], Buf("sq"), Buf("s1"), Buf("s2")
        for q in range(2):
            kb.op("dve", lambda q=q: nc.vector.memset(vv[q][:, 0:PADV], 0.0), writes=[B_vv[q]])
        wsel = {}

        def emit_proj(j):
            wa, wg = wsel[j]
            v, B_v = vv[j % 2], B_vv[j % 2]
            for bi, (t0, n) in enumerate(TB):
                ba, bg = 4 + bi % 2, 6 + bi % 2
                proj(wa, ba, t0, n)
                proj(wg, bg, t0, n)
                sg = sig[bi % 2]
                kb.op("act", lambda sg=sg, bg=bg, n=n: nc.scalar.activation(out=sg[:, 0:n], in_=PS[bg][:, 0:n], func=AF.Sigmoid,
                                                                           bias=bint[:, 8 + j:9 + j], scale=1.0),
                      reads=[B_PS[bg], B_sv], writes=[B_sig[bi % 2]])
                kb.op("dve", lambda sg=sg, ba=ba, t0=t0, n=n: nc.vector.scalar_tensor_tensor(
                    out=v[:, PADV + t0:PADV + t0 + n], in0=PS[ba][:, 0:n], scalar=bint[:, j:j + 1], in1=sg[:, 0:n],
                    op0=ALU.add, op1=ALU.mult), reads=[B_PS[ba], B_sig[bi % 2], B_sv], accw=[B_v])
            kb.op("dve", lambda: nc.vector.tensor_tensor(out=v[:, PADV:PADV + 128], in0=v[:, PADV:PADV + 128], in1=tmask, op=ALU.mult),
                  reads=[B_const, B_v], writes=[B_v])
            kb.op("act", lambda: nc.scalar.copy(out=vbf[j % 2], in_=v), reads=[B_v], writes=[B_vbf[j % 2]])
            kb.op("dve", lambda: nc.vector.tensor_tensor(
                out=dg[j % 2], in0=ident.unsqueeze(1).broadcast_to([128, PE_TAPS, 128]),
                in1=convw[:, j, 0:PE_TAPS].unsqueeze(2).broadcast_to([128, PE_TAPS, 128]), op=ALU.mult),
                reads=[B_const, B_sv], writes=[B_dg[j % 2]])

        def emit_conv(j):
            v, B_v = vv[j % 2], B_vv[j % 2]
            c = cacc[j % 2]
            B_c = B_cacc[j % 2]
            k0 = PE_TAPS
            kb.op("dve", lambda: nc.vector.tensor_scalar(out=c, in0=v[:, 2 + k0:2 + k0 + T], scalar1=convw[:, j, k0:k0 + 1], scalar2=cvec[:, 0, j:j + 1],
                                                         op0=ALU.mult, op1=ALU.add), reads=[B_v, B_sv], writes=[B_c])
            for k in range(k0 + 1, CONV_K):
                kb.op("dve", lambda k=k: nc.vector.scalar_tensor_tensor(
                    out=c, in0=v[:, 2 + k:2 + k + T], scalar=convw[:, j, k:k + 1], in1=c, op0=ALU.mult, op1=ALU.add),
                    reads=[B_v, B_sv], writes=[B_c])
            for bi, (t0, n) in enumerate(TB):
                bank = bi % 4
                for k in range(PE_TAPS):
                    kb.op("pe", lambda k=k, t0=t0, n=n, bank=bank: nc.tensor.matmul(
                        out=PS[bank][:, 0:n], lhsT=dg[j % 2][:, k, :], rhs=vbf[j % 2][:, 2 + k + t0:2 + k + t0 + n],
                        start=(k == 0), stop=(k == PE_TAPS - 1)), reads=[B_dg[j % 2], B_vbf[j % 2]],
                        writes=[B_PS[bank]] if k == 0 else (), accw=() if k == 0 else [B_PS[bank]], inc=(k == PE_TAPS - 1))
                kb.op("dve", lambda t0=t0, n=n, bank=bank: nc.vector.tensor_tensor(out=c[:, t0:t0 + n], in0=c[:, t0:t0 + n], in1=PS[bank][:, 0:n], op=ALU.add),
                      reads=[B_PS[bank], B_c], writes=[B_c])
            kb.op("act", lambda: nc.scalar.activation(out=sq, in_=c, func=AF.Square), reads=[B_c], writes=[B_sq])
            if j == 0:
                kb.op("dve", lambda: nc.vector.tensor_copy(out=s1, in_=c), reads=[B_c], writes=[B_s1])
                kb.op("dve", lambda: nc.vector.tensor_copy(out=s2, in_=sq), reads=[B_sq], writes=[B_s2])
            else:
                kb.op("dve", lambda: nc.vector.tensor_tensor(out=s1, in0=s1, in1=c, op=ALU.add), reads=[B_c, B_s1], writes=[B_s1])
                kb.op("dve", lambda: nc.vector.tensor_tensor(out=s2, in0=s2, in1=sq, op=ALU.add), reads=[B_sq, B_s2], writes=[B_s2])
            kb.dma("sp", lambda: nc.sync.dma_start(out=cT[j], in_=c), reads=[B_c], writes=[B_cT[j]])

        wsel[0] = [load_w(0), load_w(8)]
        emit_proj(0)
        for j in range(8):
            if j + 1 < 8:
                wsel[j + 1] = [load_w(j + 1), load_w(8 + j + 1)]
                emit_proj(j + 1)
            emit_conv(j)
        v = vv[0]
        B_v = B_vv[0]
        meanr = v[:, 0:T]
        rstdr = sq
        for bi, (t0, n) in enumerate(TB):
            b1, b2 = 4 + bi % 2, 6 + bi % 2
            kb.op("pe", lambda b1=b1, t0=t0, n=n: nc.tensor.matmul(out=PS[b1][:, 0:n], lhsT=ones, rhs=s1[:, t0:t0 + n], start=True, stop=True),
                  reads=[B_const, B_s1], writes=[B_PS[b1]])
            kb.op("pe", lambda b2=b2, t0=t0, n=n: nc.tensor.matmul(out=PS[b2][:, 0:n], lhsT=ones, rhs=s2[:, t0:t0 + n], start=True, stop=True),
                  reads=[B_const, B_s2], writes=[B_PS[b2]])
            kb.op("act", lambda b1=b1, t0=t0, n=n: nc.scalar.mul(out=meanr[:, t0:t0 + n], in_=PS[b1][:, 0:n], mul=1.0 / CONV_CH),
                  reads=[B_PS[b1]], accw=[B_v])
            kb.op("dve", lambda t0=t0, n=n: nc.vector.tensor_tensor(out=rstdr[:, t0:t0 + n], in0=meanr[:, t0:t0 + n], in1=meanr[:, t0:t0 + n], op=ALU.mult),
                  reads=[B_v], accw=[B_sq])
            kb.op("dve", lambda b2=b2, t0=t0, n=n: nc.vector.scalar_tensor_tensor(
                out=rstdr[:, t0:t0 + n], in0=PS[b2][:, 0:n], scalar=1.0 / CONV_CH, in1=rstdr[:, t0:t0 + n], op0=ALU.mult, op1=ALU.subtract),
                reads=[B_PS[b2], B_sq], writes=[B_sq])
        kb.op("act", lambda: nc.scalar.activation(out=rstdr, in_=rstdr, func=AF.Ln, bias=EPS, scale=1.0), reads=[B_sq], writes=[B_sq])
        kb.op("act", lambda: nc.scalar.activation(out=rstdr, in_=rstdr, func=AF.Exp, scale=-0.5), reads=[B_sq], writes=[B_sq])
        for j in range(8):
            c = cacc[j % 2]
            B_c = B_cacc[j % 2]
            kb.dma("sp", lambda c=c, j=j: nc.sync.dma_start(out=c, in_=cT[j]), reads=[B_cT[j]], writes=[B_c])
            kb.op("dve", lambda c=c: nc.vector.tensor_tensor(out=c, in0=c, in1=meanr, op=ALU.subtract), reads=[B_v, B_c], writes=[B_c])
            kb.op("dve", lambda c=c: nc.vector.tensor_tensor(out=c, in0=c, in1=rstdr, op=ALU.mult), reads=[B_sq, B_c], writes=[B_c])
            kb.op("act", lambda c=c, j=j: nc.scalar.activation(out=R[0][:, j, :], in_=c, func=AF.Silu, bias=cvec[:, 2, j:j + 1], scale=cvec[:, 1, j:j + 1]),
                  reads=[B_c, B_sv], writes=[B_R[0]] if j == 0 else (), accw=() if j == 0 else [B_R[0]])
        kb.barrier()
        ar.release(m)

    def phase_m2(l):
        m = ar.mark()
        wout = ar.alloc(BF16, [KD, D])
        B_wout = Buf("wout")
        wv = w_out_d[l].rearrange("(k p) c -> p k c", p=128)
        for k in range(KD):
            kb.dma("pool", lambda k=k: nc.gpsimd.dma_start(out=wout[:, k, :], in_=wv[:, k, :]),
                   writes=[B_wout] if k == 0 else (), accw=() if k == 0 else [B_wout])
        vec = Rf[1][:, 0:3 * D].rearrange("p (a b) -> p a b", a=3)
        B_vec = Buf("vecm2")
        kb.dma("sp", lambda: nc.sync.dma_start(out=vec, in_=vecs_in[l, 0:3].rearrange("a p d -> p a d")), writes=[B_vec])
        wr = ar.alloc(F32, [KD, 36])
        brt = ar.alloc(F32, [36])
        B_wr = Buf("wr")
        kb.dma("sp", lambda: nc.sync.dma_start(out=wr, in_=wr_d[l].rearrange("(k p) c -> p k c", p=128)), writes=[B_wr])
        kb.dma("sp", lambda: nc.sync.dma_start(out=brt, in_=br_d[l]), accw=[B_wr])
        ht = [ar.alloc(F32, [D]) for _ in range(2)]
        B_ht = [Buf("ht0"), Buf("ht1")]
        zt = ht
        B_zt = B_ht
        zbf = [ar.alloc(BF16, [D]) for _ in range(2)]
        B_zbf = [Buf("zbf0"), Buf("zbf1")]
        hT32 = Rf[1][:, 3 * D:3 * D + KD * 128].rearrange("p (a b) -> p a b", a=KD)
        B_hT32 = [Buf("hT32_%d" % g) for g in range(4)]
        small = [ar.alloc(F32, [40]) for _ in range(2)]
        B_small = [Buf("sm0"), Buf("sm1")]
        rt = ar.alloc(F32, [512])
        B_rt = Buf("rt")
        kb.op("dve", lambda: nc.vector.memset(base, 0.0), writes=[B_base])
        yreg = (0, 2)

        def load_h(i):
            kb.dma("sp", lambda: nc.sync.dma_start(out=ht[i % 2], in_=hres[i * 128:(i + 1) * 128, :]), reads=[B_hres[i]], writes=[B_ht[i % 2]])
        def emit_main(i):
            for db in range(4):
                for k in range(KD):
                    kb.op("pe", lambda k=k, db=db: nc.tensor.matmul(
                        out=PS[db][:, :], lhsT=hT_chunk(yreg, k)[:, i * 128:(i + 1) * 128], rhs=wout[:, k, db * 512:(db + 1) * 512],
                        start=(k == 0), stop=(k == KD - 1)), reads=[B_R[0], B_R[2], B_wout],
                        writes=[B_PS[db]] if k == 0 else (), accw=() if k == 0 else [B_PS[db]], inc=(k == KD - 1))
        load_h(0)
        emit_main(0)
        for i in range(NT):
            s = i % 2
            if i + 1 < NT:
                load_h(i + 1)
            z = zt[s]
            B_z = B_zt[s]
            for db in range(4):
                kb.op("dve", lambda db=db, z=z, s=s: nc.vector.scalar_tensor_tensor(
                    out=z[:, db * 512:(db + 1) * 512], in0=ht[s][:, db * 512:(db + 1) * 512], scalar=ALPHA, in1=PS[db][:, :],
                    op0=ALU.mult, op1=ALU.add), reads=[B_ht[s], B_PS[db]], writes=[B_z])
            if i + 1 < NT:
                emit_main(i + 1)
            kb.op("dve", lambda z=z: nc.vector.tensor_tensor(out=z, in0=z, in1=vec[:, 0, :], op=ALU.add), reads=[B_vec, B_z], writes=[B_z])
            ln_tile(z, B_z, vec[:, 1, :], vec[:, 2, :], B_vec, small[s], B_small[s])
            kb.dma("sp", lambda i=i, z=z: nc.sync.dma_start(out=hres[i * 128:(i + 1) * 128, :], in_=z), reads=[B_z], writes=[B_hres[i]])
            kb.op("act", lambda z=z, s=s: nc.scalar.copy(out=zbf[s], in_=z), reads=[B_z], writes=[B_zbf[s]])
            transposes_to(z, B_z, lambda g: hT32[:, 4 * g:4 * g + 4, :], B_hT32, banks=[4, 5], evac_engs=["act"])
            for k in range(KD):
                kb.op("pe", lambda k=k: nc.tensor.matmul(out=PS[6][:, 0:36], lhsT=hT32[:, k, :], rhs=wr[:, k, :], start=(k == 0), stop=(k == KD - 1)),
                      reads=[B_hT32[k // 4], B_wr], writes=[B_PS[6]] if k == 0 else (), accw=() if k == 0 else [B_PS[6]], inc=(k == KD - 1))
            routing(i, rt, B_rt, brt, B_wr)
            for kk in range(2):
                kb.dma("pool", lambda i=i, kk=kk, s=s: nc.gpsimd.indirect_dma_start(
                    out=xs, out_offset=bass.IndirectOffsetOnAxis(ap=dest_i[:, i, kk:kk + 1], axis=0), in_=zbf[s], in_offset=None),
                    reads=[B_zbf[s], B_route[i]], accw=[B_xs])
        kb.barrier()
        ar.release(m)

    def routing(i, rt, B_rt, brt, B_wr):
        def V(a, n):
            return rt[:, a:a + n]
        Lg = V(0, 36)
        gmax, gsum, gp = V(36, 1), V(37, 1), V(38, 1)
        gmask = V(40, 4)
        gexp = V(44, 4)
        tmp32 = V(48, 32)
        esel = V(80, 8)
        m1, m2, ng = V(88, 1), V(89, 1), V(90, 1)
        mask1, mask2 = V(96, 8), V(104, 8)
        esel2 = V(112, 8)
        dlt, p1, p2 = V(120, 1), V(121, 1), V(122, 1)
        oh1, oh2, oh = V(128, 32), V(160, 32), V(192, 32)
        slot = V(224, 32)
        valid = V(256, 32)
        tmpb = V(288, 32)

        def dve(fn, w=True):
            kb.op("dve", fn, reads=[B_rt], writes=[B_rt])

        kb.op("dve", lambda: nc.vector.tensor_tensor(out=Lg, in0=PS[6][:, 0:36], in1=brt, op=ALU.add), reads=[B_PS[6], B_wr], writes=[B_rt])
        dve(lambda: nc.vector.tensor_reduce(out=gmax, in_=Lg[:, 0:4], axis=AX.X, op=ALU.max))
        dve(lambda: nc.vector.tensor_scalar(out=gmask, in0=Lg[:, 0:4], scalar1=gmax, scalar2=None, op0=ALU.is_equal))
        dve(lambda: nc.vector.tensor_scalar(out=ng, in0=gmax, scalar1=-1.0, scalar2=None, op0=ALU.mult))
        kb.op("act", lambda: nc.scalar.activation(out=gexp, in_=Lg[:, 0:4], func=AF.Exp, bias=ng, scale=1.0), reads=[B_rt], writes=[B_rt])
        dve(lambda: nc.vector.tensor_reduce(out=gsum, in_=gexp, axis=AX.X, op=ALU.add))
        dve(lambda: nc.vector.reciprocal(out=gp, in_=gsum))
        le = Lg[:, 4:36].rearrange("p (g j) -> p g j", g=4)
        dve(lambda: nc.vector.tensor_tensor(out=tmp32.rearrange("p (g j) -> p g j", g=4), in0=le,
                                            in1=gmask.unsqueeze(2).broadcast_to([128, 4, 8]), op=ALU.mult))
        dve(lambda: nc.vector.tensor_reduce(out=esel, in_=tmp32.rearrange("p (g j) -> p j g", g=4), axis=AX.X, op=ALU.add))
        dve(lambda: nc.vector.tensor_reduce(out=m1, in_=esel, axis=AX.X, op=ALU.max))
        dve(lambda: nc.vector.tensor_scalar(out=mask1, in0=esel, scalar1=m1, scalar2=None, op0=ALU.is_equal))
        dve(lambda: nc.vector.scalar_tensor_tensor(out=esel2, in0=mask1, scalar=-1e30, in1=esel, op0=ALU.mult, op1=ALU.add))
        dve(lambda: nc.vector.tensor_reduce(out=m2, in_=esel2, axis=AX.X, op=ALU.max))
        dve(lambda: nc.vector.tensor_scalar(out=mask2, in0=esel2, scalar1=m2, scalar2=None, op0=ALU.is_equal))
        dve(lambda: nc.vector.tensor_tensor(out=dlt, in0=m2, in1=m1, op=ALU.subtract))
        kb.op("act", lambda: nc.scalar.activation(out=dlt, in_=dlt, func=AF.Exp), reads=[B_rt], writes=[B_rt])
        dve(lambda: nc.vector.tensor_scalar(out=p1, in0=dlt, scalar1=1.0, scalar2=None, op0=ALU.add))
        dve(lambda: nc.vector.reciprocal(out=p1, in_=p1))
        dve(lambda: nc.vector.tensor_tensor(out=p2, in0=dlt, in1=p1, op=ALU.mult))
        kb.op("dve", lambda: nc.vector.tensor_tensor(out=gates[:, i, 0:1], in0=p1, in1=gp, op=ALU.mult), reads=[B_rt], writes=[B_route[i]])
        kb.op("dve", lambda: nc.vector.tensor_tensor(out=gates[:, i, 1:2], in0=p2, in1=gp, op=ALU.mult), reads=[B_rt], accw=[B_route[i]])
        for ohx, mk in ((oh1, mask1), (oh2, mask2)):
            dve(lambda ohx=ohx, mk=mk: nc.vector.tensor_tensor(out=ohx.rearrange("p (g j) -> p g j", g=4),
                                                             in0=gmask.unsqueeze(2).broadcast_to([128, 4, 8]),
                                                             in1=mk.unsqueeze(1).broadcast_to([128, 4, 8]), op=ALU.mult))
        dve(lambda: nc.vector.tensor_tensor(out=oh, in0=oh1, in1=oh2, op=ALU.add))
        if i == 0:
            kb.op("dve", lambda: nc.vector.tensor_scalar(out=oh, in0=oh, scalar1=tokv[:, 0:1], scalar2=None, op0=ALU.mult),
                  reads=[B_rt, B_const], writes=[B_rt])
        kb.op("pe", lambda: nc.tensor.matmul(out=PS[7][:, 0:32], lhsT=tri, rhs=oh, start=True, stop=True), reads=[B_rt, B_const], writes=[B_PS[7]])
        kb.op("pe", lambda: nc.tensor.matmul(out=PS[7][:, 32:64], lhsT=ones, rhs=oh, start=True, stop=True), reads=[B_rt, B_const], accw=[B_PS[7]])
        kb.op("dve", lambda: nc.vector.tensor_tensor(out=slot, in0=PS[7][:, 0:32], in1=base, op=ALU.add), reads=[B_PS[7], B_base, B_rt], writes=[B_rt])
        kb.op("dve", lambda: nc.vector.tensor_tensor(out=base, in0=base, in1=PS[7][:, 32:64], op=ALU.add), reads=[B_PS[7], B_base], writes=[B_base])
        dve(lambda: nc.vector.tensor_scalar(out=valid, in0=slot, scalar1=float(CAP), scalar2=None, op0=ALU.is_lt))
        kb.op("dve", lambda: nc.vector.tensor_tensor(out=slot, in0=slot, in1=ecvec, op=ALU.add), reads=[B_rt, B_const], writes=[B_rt])
        if i == 0:
            kb.op("dve", lambda: nc.vector.tensor_scalar(out=valid, in0=valid, scalar1=tokv[:, 0:1], scalar2=None, op0=ALU.mult),
                  reads=[B_rt, B_const], writes=[B_rt])
        kb.op("dve", lambda: nc.vector.tensor_scalar(out=slot, in0=slot, scalar1=tokv[:, 2:3], scalar2=None, op0=ALU.add),
              reads=[B_rt, B_const], writes=[B_rt])
        dve(lambda: nc.vector.tensor_tensor(out=slot, in0=slot, in1=valid, op=ALU.mult))
        kb.op("dve", lambda: nc.vector.tensor_scalar(out=slot, in0=slot, scalar1=tokv[:, 1:2], scalar2=None, op0=ALU.add),
              reads=[B_rt, B_const], writes=[B_rt])
        for kk, ohx in ((0, oh1), (1, oh2)):
            dve(lambda ohx=ohx: nc.vector.tensor_tensor(out=tmpb, in0=ohx, in1=slot, op=ALU.mult))
            kb.op("dve", lambda kk=kk: nc.vector.tensor_reduce(out=dest_f[:, i, kk:kk + 1], in_=tmpb, axis=AX.X, op=ALU.add),
                  reads=[B_rt], accw=[B_route[i]])
        kb.op("dve", lambda: nc.vector.tensor_copy(out=dest_i[:, i, :], in_=dest_f[:, i, :]), reads=[B_route[i]], accw=[B_route[i]])

    def phase_experts(l):
        m = ar.mark()
        NB = 2
        def rview(i, off_bf, a, b):
            return Rf[i].bitcast(BF16)[:, off_bf:off_bf + a * b].rearrange("p (a b) -> p a b", a=a)
        wg = [rview(i, 0, KD, EH) for i in range(NB)]
        wu = [rview(i, KD * EH, KD, EH) for i in range(NB)]
        wd = [rview(2, i * 4 * D, 4, D) for i in range(NB)]
        xT = [ar.alloc(BF16, [KD, CAP]) for _ in range(NB)]
        B_wg = [Buf("wg%d" % i) for i in range(NB)]
        B_wu = [Buf("wu%d" % i) for i in range(NB)]
        B_wd = [Buf("wd%d" % i) for i in range(NB)]
        B_xT = [Buf("xT%d" % i) for i in range(NB)]
        sgt = [ar.alloc(F32, [CAP]) for _ in range(2)]
        B_sgt = [Buf("sg0"), Buf("sg1")]
        hb = [ar.alloc(BF16, [4, CAP]) for _ in range(2)]
        B_hb = [Buf("hb0"), Buf("hb1")]
        yt = [ar.alloc(F32, [D]) for _ in range(2)]
        B_yt = [Buf("yt0"), Buf("yt1")]

        def load_expert(e):
            s = e % NB
            gv = wg_d[l, e].rearrange("(k p) c -> p k c", p=128)
            uv = wu_d[l, e].rearrange("(k p) c -> p k c", p=128)
            dv = wd_d[l, e].rearrange("(k p) c -> p k c", p=128)
            for q in range(4):
                kb.dma("pool", lambda q=q: nc.gpsimd.dma_start(out=wg[s][:, 4 * q:4 * q + 4, :], in_=gv[:, 4 * q:4 * q + 4, :]),
                       writes=[B_wg[s]] if q == 0 else (), accw=() if q == 0 else [B_wg[s]])
            for q in range(4):
                kb.dma("pool", lambda q=q: nc.gpsimd.dma_start(out=wu[s][:, 4 * q:4 * q + 4, :], in_=uv[:, 4 * q:4 * q + 4, :]),
                       writes=[B_wu[s]] if q == 0 else (), accw=() if q == 0 else [B_wu[s]])
            for q in range(4):
                kb.dma("pool", lambda q=q: nc.gpsimd.dma_start(out=wd[s][:, q, :], in_=dv[:, q, :]),
                       writes=[B_wd[s]] if q == 0 else (), accw=() if q == 0 else [B_wd[s]])
            for k in range(KD):
                kb.dma("sp", lambda k=k: nc.sync.dma_start_transpose(out=xT[s][:, k, :], in_=xs[e * CAP:(e + 1) * CAP, k * 128:(k + 1) * 128]),
                       reads=[B_xs], writes=[B_xT[s]] if k == 0 else (), accw=() if k == 0 else [B_xT[s]])

        load_expert(0)
        for e in range(NEXP):
            s = e % NB
            if e + 1 < NEXP:
                load_expert(e + 1)
            hs = e % 2
            for hc in range(4):
                bg, bu = (hc % 2), 2 + (hc % 2)
                for k in range(KD):
                    kb.op("pe", lambda k=k, hc=hc, bg=bg: nc.tensor.matmul(out=PS[bg][:, 0:CAP], lhsT=wg[s][:, k, hc * 128:(hc + 1) * 128],
                                                                           rhs=xT[s][:, k, :], start=(k == 0), stop=(k == KD - 1)),
                          reads=[B_wg[s], B_xT[s]], writes=[B_PS[bg]] if k == 0 else (), accw=() if k == 0 else [B_PS[bg]], inc=(k == KD - 1))
                for k in range(KD):
                    kb.op("pe", lambda k=k, hc=hc, bu=bu: nc.tensor.matmul(out=PS[bu][:, 0:CAP], lhsT=wu[s][:, k, hc * 128:(hc + 1) * 128],
                                                                           rhs=xT[s][:, k, :], start=(k == 0), stop=(k == KD - 1)),
                          reads=[B_wu[s], B_xT[s]], writes=[B_PS[bu]] if k == 0 else (), accw=() if k == 0 else [B_PS[bu]], inc=(k == KD - 1))
                sg = sgt[hc % 2]
                kb.op("act", lambda sg=sg, bg=bg: nc.scalar.activation(out=sg, in_=PS[bg][:, 0:CAP], func=AF.Silu),
                      reads=[B_PS[bg]], writes=[B_sgt[hc % 2]])
                kb.op("dve", lambda sg=sg, bu=bu, hc=hc, hs=hs: nc.vector.tensor_tensor(out=hb[hs][:, hc, :], in0=sg, in1=PS[bu][:, 0:CAP], op=ALU.mult),
                      reads=[B_sgt[hc % 2], B_PS[bu]], writes=[B_hb[hs]] if hc == 0 else (), accw=() if hc == 0 else [B_hb[hs]])
            for st in range(CAP // 128):
                ys_s = (e * (CAP // 128) + st) % 2
                for db in range(4):
                    bank = 4 + db
                    for hc in range(4):
                        kb.op("pe", lambda hc=hc, db=db, st=st, bank=bank: nc.tensor.matmul(
                            out=PS[bank][:, :], lhsT=hb[hs][:, hc, st * 128:(st + 1) * 128], rhs=wd[s][:, hc, db * 512:(db + 1) * 512],
                            start=(hc == 0), stop=(hc == 3)), reads=[B_hb[hs], B_wd[s]],
                            writes=[B_PS[bank]] if hc == 0 else (), accw=() if hc == 0 else [B_PS[bank]], inc=(hc == 3))
                    eng = "act" if db % 2 == 0 else "dve"
                    if eng == "act":
                        kb.op("act", lambda db=db, bank=bank, ys_s=ys_s: nc.scalar.copy(out=yt[ys_s][:, db * 512:(db + 1) * 512], in_=PS[bank][:, :]),
                              reads=[B_PS[bank]], writes=[B_yt[ys_s]] if db == 0 else (), accw=() if db == 0 else [B_yt[ys_s]])
                    else:
                        kb.op("dve", lambda db=db, bank=bank, ys_s=ys_s: nc.vector.tensor_copy(out=yt[ys_s][:, db * 512:(db + 1) * 512], in_=PS[bank][:, :]),
                              reads=[B_PS[bank]], accw=[B_yt[ys_s]])
                r0 = e * CAP + st * 128
                kb.dma("sp", lambda r0=r0, ys_s=ys_s: nc.sync.dma_start(out=ys[r0:r0 + 128, :], in_=yt[ys_s]), reads=[B_yt[ys_s]], accw=[B_ys])
        kb.barrier()
        ar.release(m)

    def phase_f5(l, last):
        m = ar.mark()
        vec = ar.alloc(F32, [2, D])
        B_vec = Buf("vecf5")
        kb.dma("sp", lambda: nc.sync.dma_start(out=vec, in_=vecs_in[l, 3:5].rearrange("a p d -> p a d")), writes=[B_vec])
        ht = [ar.alloc(F32, [D]) for _ in range(2)]
        B_ht = [Buf("ht0"), Buf("ht1")]
        y1 = [ar.alloc(F32, [D]) for _ in range(2)]
        y2 = [ar.alloc(F32, [D]) for _ in range(2)]
        B_y1 = [Buf("y1_0"), Buf("y1_1")]
        B_y2 = [Buf("y2_0"), Buf("y2_1")]
        small = [ar.alloc(F32, [40]) for _ in range(2)]
        B_small = [Buf("sm0"), Buf("sm1")]
        def load_t(i):
            s = i % 2
            kb.dma("sp", lambda: nc.sync.dma_start(out=ht[s], in_=hres[i * 128:(i + 1) * 128, :]), reads=[B_hres[i]], writes=[B_ht[s]])
            kb.dma("pool", lambda: nc.gpsimd.indirect_dma_start(
                out=y1[s], out_offset=None, in_=ys, in_offset=bass.IndirectOffsetOnAxis(ap=dest_i[:, i, 0:1], axis=0)),
                reads=[B_ys, B_route[i]], writes=[B_y1[s]])
            kb.dma("pool", lambda: nc.gpsimd.indirect_dma_start(
                out=y2[s], out_offset=None, in_=ys, in_offset=bass.IndirectOffsetOnAxis(ap=dest_i[:, i, 1:2], axis=0)),
                reads=[B_ys, B_route[i]], writes=[B_y2[s]])
        load_t(0)
        for i in range(NT):
            s = i % 2
            if i + 1 < NT:
                load_t(i + 1)
            z = ht[s]
            B_z = B_ht[s]
            kb.op("pool", lambda z=z, s=s, i=i: nc.gpsimd.tensor_scalar(out=y1[s], in0=y1[s], scalar1=gates[:, i, 0:1], scalar2=None, op0=ALU.mult),
                  reads=[B_route[i], B_y1[s]], writes=[B_y1[s]])
            kb.op("dve", lambda z=z, s=s: nc.vector.scalar_tensor_tensor(out=z, in0=z, scalar=ALPHA, in1=y1[s], op0=ALU.mult, op1=ALU.add),
                  reads=[B_y1[s], B_z], writes=[B_z])
            kb.op("dve", lambda z=z, s=s, i=i: nc.vector.scalar_tensor_tensor(out=z, in0=y2[s], scalar=gates[:, i, 1:2], in1=z, op0=ALU.mult, op1=ALU.add),
                  reads=[B_y2[s], B_route[i], B_z], writes=[B_z])
            ln_tile(z, B_z, vec[:, 0, :], vec[:, 1, :], B_vec, small[s], B_small[s])
            if last:
                if i >= 1:
                    kb.dma("sp", lambda i=i, z=z: nc.sync.dma_start(out=out_d[(i - 1) * 128:i * 128, :], in_=z), reads=[B_z], accw=[B_out])
            else:
                kb.dma("sp", lambda i=i, z=z: nc.sync.dma_start(out=hres[i * 128:(i + 1) * 128, :], in_=z), reads=[B_z], writes=[B_hres[i]])

                def dst(g, i=i):
                    k0 = 4 * g
                    return R[k0 // 8][:, (k0 % 8):(k0 % 8) + 4, i * 128:(i + 1) * 128]
                transposes_to(z, B_z, dst, [B_R[0], B_R[0], B_R[1], B_R[1]], banks=[0, 1, 2, 3], evac_engs=["act", "dve"])
            if debug and last is False and False:
                pass
        kb.barrier()
        ar.release(m)

    B_out = Buf("out")
    stages = []
    phase_input_ln()
    if stop_after == "exp_dma":
        mm = ar.mark()
        wchx = [ar.alloc(BF16, [KD, 128]) for _ in range(4)]
        B_wx = [Buf("wx%d" % i) for i in range(4)]
        wv_x = w_in_d[0].rearrange("(k p) c -> p k c", p=128)
        for cc in range(24):
            kb.dma("pool", lambda cc=cc: nc.gpsimd.dma_start(out=wchx[cc % 4], in_=wv_x[:, :, cc * 128:(cc + 1) * 128]), writes=[B_wx[cc % 4]])
            import os
            if os.environ.get("EXP_SERIAL"):
                kb.wait_all("pool", B_wx)
        kb.barrier()
        ar.release(mm)
    done = (stop_after in ("ln0", "exp_dma"))
    if done and debug:
        for ri in range(3):
            kb.dma("sp", lambda ri=ri: nc.sync.dma_start(out=dbg_y[ri], in_=R[ri]), reads=[B_R[ri]], writes=[B_out])
    for l in range(depth):
        if done:
            break
        phase_m1(l)
        if stop_after == "m1_%d" % l:
            for ri in range(3):
                kb.dma("sp", lambda ri=ri: nc.sync.dma_start(out=dbg_y[ri], in_=R[ri]), reads=[B_R[ri]], writes=[B_out])
            break
        phase_m2(l)
        if stop_after == "m2_%d" % l:
            break
        if debug:
            kb.dma("sp", lambda: nc.sync.dma_start(out=dbg_r[0], in_=dest_f), reads=B_route, writes=[B_out])
            kb.dma("sp", lambda: nc.sync.dma_start(out=dbg_r[1], in_=gates), reads=B_route, writes=[B_out])
        phase_experts(l)
        if stop_after == "ex_%d" % l:
            break
        last = (l == depth - 1) and not debug
        phase_f5(l, last)
        if stop_after == "f5_%d" % l:
            break
    kb.wait_all("sp", [B_out, B_xs, B_ys] + B_cT + B_hres)
    kb.wait_all("sp", B_R + B_PS)
    kb.close()
    for cm in reversed(ps_cms):
        cm.__exit__(None, None, None)
    arena_cm.__exit__(None, None, None)
    return nc


def _rep(v):
    return np.ascontiguousarray(np.broadcast_to(np.asarray(v, np.float32)[None, :], (128, v.shape[-1])))


def prepare_inputs(inp):
    f = lambda a: np.ascontiguousarray(np.asarray(a, dtype=np.float32))
    x = f(inp["x"])[0]
    meta = f(inp["meta_tokens"])
    stream = np.concatenate([np.zeros((HALO - NMETA, D), np.float32), meta, x], axis=0)
    L = DEPTH
    lnin = np.stack([_rep(f(inp["ln_in_g"])), _rep(f(inp["ln_in_b"]))])
    vecs = np.stack([np.stack([_rep(f(inp[k])[l]) for k in ("b_out", "ln_mix_g", "ln_mix_b", "ln_ffn_g", "ln_ffn_b")]) for l in range(L)])
    b_in_t = np.ascontiguousarray(f(inp["b_in"]).reshape(L, 24, 128).transpose(0, 2, 1))
    conv_w_t = np.ascontiguousarray(f(inp["conv_w"])[:, :, 0, :].reshape(L, CONV_K, 8, 128).transpose(0, 3, 2, 1))
    cv = [f(inp["conv_b"]), f(inp["conv_ln_g"]), f(inp["conv_ln_b"]), f(inp["pool_b"]).reshape(L, 1024), f(inp["pool_scale"])]
    cvec = np.ascontiguousarray(np.stack([c.reshape(L, 8, 128).transpose(0, 2, 1) for c in cv], axis=2))
    wr = np.ascontiguousarray(np.concatenate([f(inp["router_group_w"]), f(inp["router_expert_w"])], axis=2))
    brv = np.concatenate([f(inp["router_group_b"]), f(inp["router_expert_b"])], axis=1)
    br = np.ascontiguousarray(np.broadcast_to(brv[:, None, :], (L, 128, 36)))
    ident = np.eye(128, dtype=np.float32)
    tri = np.triu(np.ones((128, 128), np.float32), 1)
    ones = np.ones((128, 128), np.float32)
    consts = np.ascontiguousarray(np.stack([ident, tri, ones], axis=1))
    ecvec = np.ascontiguousarray(np.broadcast_to((np.arange(NEXP, dtype=np.float32) * CAP)[None, :], (128, NEXP)))
    shared = dict(consts=consts, ecvec=ecvec, lnin=lnin, vecs=vecs, w_in=f(inp["w_in"]), w_out=f(inp["w_out"]),
                  pool_w=f(inp["pool_w"]), b_in_t=b_in_t, conv_w_t=conv_w_t, cvec=cvec, wr=wr, br=br,
                  w_gate=f(inp["w_gate"]), w_up=f(inp["w_up"]), w_down=f(inp["w_down"]))
    maps = []
    for c in range(NCORES):
        r0 = c * TOK_OUT
        xc = np.ascontiguousarray(stream[r0:r0 + T])
        tm = np.ones((128,), np.float32)
        ic = np.stack([np.full((128,), 1.0 / w, np.float32) for w in POOL_W])
        if c == 0:
            tm[:HALO - NMETA] = 0.0
            p = np.arange(128) - (HALO - NMETA)
            for gi, w in enumerate(POOL_W):
                ic[gi] = np.where(p >= 0, 1.0 / np.minimum(w, np.maximum(p, 0) + 1), 1.0 / w).astype(np.float32)
        d = dict(shared)
        d["x"] = xc
        d["tmask"] = np.ascontiguousarray(np.broadcast_to(tm[None, :], (128, 128)))
        d["invcnt"] = np.ascontiguousarray(np.broadcast_to(ic[None, :, :], (128, 4, 128)))
        tv = np.ones((128, 4), np.float32)
        tv[:, 0] = tm
        tv[:, 1] = TRASH + np.arange(128)
        tv[:, 2] = -(TRASH + np.arange(128))
        d["tokv"] = tv
        maps.append(d)
    return maps


_NC_CACHE = {}


def kernel(**inputs):
    maps = prepare_inputs(inputs)
    if "nc" not in _NC_CACHE:
        _NC_CACHE["nc"] = build_program()
    nc = _NC_CACHE["nc"]
    res = run_bass_kernel_spmd(nc, maps, core_ids=list(range(NCORES)))
    out = np.concatenate([np.asarray(r["out"], dtype=np.float32) for r in res.results], axis=0)
    return out.reshape(1, SEQ, D)
```

```python
import numpy as np
import ml_dtypes
import concourse.bass as bass
import concourse.mybir as mybir
from concourse.bass_utils import run_bass_kernel_spmd

F32 = mybir.dt.float32
BF16 = mybir.dt.bfloat16
I32 = mybir.dt.int32
AF = mybir.ActivationFunctionType
ALU = mybir.AluOpType
AX = mybir.AxisListType

D = 2048
SEQ = 16384
DEPTH = 4
NMETA = 16
NCORES = 8
TOK_OUT = SEQ // NCORES
HALO = 128
T = TOK_OUT + HALO
NT = T // 128
KD = D // 128
CONV_CH = 1024
CONV_K = 31
NEXP = 32
EH = 512
CAP = 256
NSLOT = NEXP * CAP
TRASH = NSLOT
ALPHA = float((2.0 * DEPTH) ** 0.25)
EPS = 1e-5
POOL_W = (2, 4, 8, 16)
TB = [(0, 512), (512, 512), (1024, 512), (1536, 512), (2048, 128)]


PHASE_MARKS = []


def merge(dst, src):
    for k, (s, v) in src.items():
        if k not in dst or dst[k][1] < v:
            dst[k] = (s, v)


class Buf:
    def __init__(self, name=""):
        self.name = name
        self.w = {}
        self.r = {}


class KB:
    def __init__(self, nc):
        self.nc = nc
        self.eng = {"pe": nc.tensor, "act": nc.scalar, "dve": nc.vector, "pool": nc.gpsimd, "sp": nc.sync}
        self.sem = {}
        self.cnt = {}
        self.waited = {e: {} for e in self.eng}
        self.emitted = {e: 0 for e in self.eng}
        self._cms = []
        for e in ("pe", "act", "dve", "pool"):
            cm = nc.semaphore("p_" + e)
            self.sem[e] = cm.__enter__()
            self._cms.append(cm)
            self.cnt[e] = 0
        self.ring = {}
        self.ringpos = {}
        for q, n in (("sp", 20), ("pool", 28), ("act", 8)):
            lst = []
            for i in range(n):
                cm = nc.semaphore("d_%s%d" % (q, i))
                lst.append([cm.__enter__(), 0])
                self._cms.append(cm)
            self.ring[q] = lst
            self.ringpos[q] = 0

    def close(self):
        for cm in reversed(self._cms):
            cm.__exit__(None, None, None)

    def _wait(self, e, deps):
        w = self.waited[e]
        for k, (s, v) in deps.items():
            if v > 0 and w.get(k, 0) < v:
                self.eng[e].wait_ge(s, v)
                self.emitted[e] += 1
                w[k] = v

    def _deps(self, e, reads, writes, accw):
        deps = {}
        for b in reads:
            merge(deps, b.w)
        for b in writes:
            merge(deps, b.w)
            merge(deps, b.r)
        for b in accw:
            merge(deps, b.r)
        if e == "pe":
            deps.pop("pe", None)
        return deps

    def _post(self, tok, reads, writes, accw):
        for b in reads:
            merge(b.r, tok)
        for b in writes:
            b.w = dict(tok)
            b.r = {}
        for b in accw:
            merge(b.w, tok)

    def op(self, e, fn, reads=(), writes=(), accw=(), inc=True):
        self._wait(e, self._deps(e, reads, writes, accw))
        inst = fn()
        self.emitted[e] += 1
        if inc:
            self.cnt[e] += 1
            inst.then_inc(self.sem[e], 1)
            tok = {e: (self.sem[e], self.cnt[e])}
        else:
            tok = {e: (self.sem[e], self.cnt[e] + 1)}
        self._post(tok, reads, writes, accw)
        return tok

    def dma(self, q, fn, reads=(), writes=(), accw=()):
        self._wait(q, self._deps(q, reads, writes, accw))
        lst = self.ring[q]
        i = self.ringpos[q]
        self.ringpos[q] = (i + 1) % len(lst)
        ent = lst[i]
        key = "d_%s%d" % (q, i)
        self._wait(q, {key: (ent[0], ent[1])})
        inst = fn()
        self.emitted[q] += 1
        ent[1] += 16
        inst.then_inc(ent[0], 16)
        tok = {key: (ent[0], ent[1])}
        self._post(tok, reads, writes, accw)
        return tok

    def barrier(self):
        PHASE_MARKS.append(dict(self.emitted))
        deps = {}
        for e in ("pe", "act", "dve", "pool"):
            deps[e] = (self.sem[e], self.cnt[e])
        for q, lst in self.ring.items():
            for i, ent in enumerate(lst):
                deps["d_%s%d" % (q, i)] = (ent[0], ent[1])
        for e in ("pe", "act", "dve", "pool", "sp"):
            self._wait(e, deps)

    def wait_all(self, e, bufs):
        deps = {}
        for b in bufs:
            merge(deps, b.w)
            merge(deps, b.r)
        self._wait(e, deps)


class Arena:
    def __init__(self, ap_f32, nbytes):
        self.ap = ap_f32
        self.nbytes = nbytes
        self.off = 0

    def mark(self):
        return self.off

    def release(self, m):
        self.off = m

    def alloc(self, dtype, free_shape):
        esz = 4 if dtype in (F32, I32) else 2
        n = int(np.prod(free_shape))
        nb = (n * esz + 31) // 32 * 32
        assert self.off + nb <= self.nbytes, ("arena overflow", self.off, nb, self.nbytes)
        v = self.ap[:, self.off // 4:(self.off + nb) // 4]
        if dtype != F32:
            v = v.bitcast(dtype)
        v = v[:, 0:n]
        self.off += nb
        if len(free_shape) == 2:
            v = v.rearrange("p (a b) -> p a b", a=free_shape[0])
        elif len(free_shape) == 3:
            v = v.rearrange("p (a b c) -> p a b c", a=free_shape[0], b=free_shape[1])
        return v


def build_program(depth=DEPTH, stop_after=None, debug=False):
    nc = bass.Bass("TRN2", target_bir_lowering=False)
    L = DEPTH

    def din(name, shape, dt=F32):
        return nc.dram_tensor(name, list(shape), dt, kind="ExternalInput").ap()

    x_in = din("x", [T, D])
    tmask_in = din("tmask", [128, 128])
    invcnt_in = din("invcnt", [128, 4, 128])
    tokv_in = din("tokv", [128, 4])
    consts_in = din("consts", [128, 3, 128])
    ecvec_in = din("ecvec", [128, NEXP])
    lnin_in = din("lnin", [2, 128, D])
    vecs_in = din("vecs", [L, 5, 128, D])
    w_in_d = din("w_in", [L, D, 3072])
    w_out_d = din("w_out", [L, D, D])
    pool_w_d = din("pool_w", [L, 4, 256, 256])
    bin_d = din("b_in_t", [L, 128, 24])
    convw_d = din("conv_w_t", [L, 128, 8, CONV_K])
    cvec_d = din("cvec", [L, 128, 5, 8])
    wr_d = din("wr", [L, D, 36])
    br_d = din("br", [L, 128, 36])
    wg_d = din("w_gate", [L, NEXP, D, EH])
    wu_d = din("w_up", [L, NEXP, D, EH])
    wd_d = din("w_down", [L, NEXP, EH, D])
    out_d = nc.dram_tensor("out", [TOK_OUT, D], F32, kind="ExternalOutput").ap()
    skind = "ExternalOutput" if debug else "Internal"
    hres = nc.dram_tensor("hres", [T, D], F32, kind=skind).ap()
    cT = nc.dram_tensor("cT", [8, 128, T], F32, kind=skind).ap()
    dbg_y = nc.dram_tensor("dbg_y", [3, 128, 8, T], BF16, kind=skind).ap() if debug else None
    dbg_r = nc.dram_tensor("dbg_r", [2, 128, NT, 2], F32, kind=skind).ap() if debug else None
    xs = nc.dram_tensor("xs", [NSLOT + 128, D], BF16, kind="Internal").ap()
    ys = nc.dram_tensor("ys", [NSLOT + 128, D], F32, kind="Internal").ap()
    B_hres = [Buf("hres%d" % i) for i in range(NT)]
    B_cT = [Buf("cT%d" % j) for j in range(8)]
    B_xs = Buf("xs")
    B_ys = Buf("ys")

    ARENA_BYTES = 212480
    arena_cm = nc.sbuf_tensor("arena", [128, ARENA_BYTES // 4], F32)
    arena_t = arena_cm.__enter__()
    ar = Arena(arena_t[:, :], ARENA_BYTES)
    ps_cms = [nc.psum_tensor("ps%d" % i, [128, 512], F32) for i in range(8)]
    PS = [cm.__enter__() for cm in ps_cms]
    B_PS = [Buf("ps%d" % i) for i in range(8)]
    kb = KB(nc)

    Rf = [ar.alloc(F32, [4 * T]) for _ in range(3)]
    R = [r.bitcast(BF16).rearrange("p (a b) -> p a b", a=8) for r in Rf]
    B_R = [Buf("R%d" % i) for i in range(3)]
    consts = ar.alloc(F32, [3, 128])
    ident, tri, ones = consts[:, 0, :], consts[:, 1, :], consts[:, 2, :]
    ecvec = ar.alloc(F32, [NEXP])
    tmask = ar.alloc(F32, [128])
    invcnt = ar.alloc(F32, [4, 128])
    tokv = ar.alloc(F32, [4])
    dest_f = ar.alloc(F32, [NT, 2])
    dest_i = ar.alloc(I32, [NT, 2])
    gates = ar.alloc(F32, [NT, 2])
    base = ar.alloc(F32, [NEXP])
    B_const = Buf("const")
    B_route = [Buf("route%d" % i) for i in range(NT)]
    B_base = Buf("base")
    kb.dma("sp", lambda: nc.sync.dma_start(out=consts, in_=consts_in), writes=[B_const])
    kb.dma("sp", lambda: nc.sync.dma_start(out=ecvec, in_=ecvec_in), accw=[B_const])
    kb.dma("sp", lambda: nc.sync.dma_start(out=tmask, in_=tmask_in), accw=[B_const])
    kb.dma("sp", lambda: nc.sync.dma_start(out=invcnt, in_=invcnt_in), accw=[B_const])
    kb.dma("sp", lambda: nc.sync.dma_start(out=tokv, in_=tokv_in), accw=[B_const])
    persist_mark = ar.mark()

    def hT_chunk(regions, k):
        return R[regions[k // 8]][:, k % 8, :]

    def ln_tile(z, B_z, gv, bv, B_vec, small, B_small):
        st = small[:, 0:24].rearrange("p (a b) -> p a b", a=4)
        mv = small[:, 24:26]
        for c in range(4):
            kb.op("dve", lambda c=c: nc.vector.bn_stats(out=st[:, c, :], in_=z[:, c * 512:(c + 1) * 512]),
                  reads=[B_z], accw=[B_small] if c else (), writes=() if c else [B_small])
        kb.op("dve", lambda: nc.vector.bn_aggr(out=mv, in_=st), reads=[B_small], accw=[B_small])
        kb.op("act", lambda: nc.scalar.activation(out=small[:, 26:27], in_=small[:, 25:26], func=AF.Ln, bias=EPS, scale=1.0),
              reads=[B_small], accw=[B_small])
        kb.op("act", lambda: nc.scalar.activation(out=small[:, 27:28], in_=small[:, 26:27], func=AF.Exp, scale=-0.5),
              reads=[B_small], accw=[B_small])
        kb.op("dve", lambda: nc.vector.tensor_scalar(out=z, in0=z, scalar1=small[:, 24:25], scalar2=small[:, 27:28],
                                                     op0=ALU.subtract, op1=ALU.mult), reads=[B_small, B_z], writes=[B_z])
        kb.op("dve", lambda: nc.vector.tensor_tensor(out=z, in0=z, in1=gv, op=ALU.mult), reads=[B_vec, B_z], writes=[B_z])
        kb.op("dve", lambda: nc.vector.tensor_tensor(out=z, in0=z, in1=bv, op=ALU.add), reads=[B_vec, B_z], writes=[B_z])

    def transposes_to(z, B_z, dst_fn, B_dst_list, banks, evac_engs):
        for g in range(4):
            bk = banks[g % len(banks)]
            for q in range(4):
                k = 4 * g + q
                kb.op("pe", lambda k=k, q=q, bk=bk: nc.tensor.transpose(out=PS[bk][:, q * 128:(q + 1) * 128],
                                                                         in_=z[:, k * 128:(k + 1) * 128], identity=ident),
                      reads=[B_z, B_const], writes=[B_PS[bk]] if q == 0 else (), accw=() if q == 0 else [B_PS[bk]])
            e = evac_engs[g % len(evac_engs)]
            src = PS[bk][:, :].rearrange("p (a b) -> p a b", a=4)
            if e == "act":
                kb.op("act", lambda g=g, src=src: nc.scalar.copy(out=dst_fn(g), in_=src), reads=[B_PS[bk]], accw=[B_dst_list[g]])
            else:
                kb.op("dve", lambda g=g, src=src: nc.vector.tensor_copy(out=dst_fn(g), in_=src), reads=[B_PS[bk]], accw=[B_dst_list[g]])

    def phase_input_ln():
        m = ar.mark()
        zt0 = ar.alloc(F32, [D])
        B_zt0 = Buf("zt0")
        kb.op("dve", lambda: nc.vector.memset(zt0, 0.0), writes=[B_zt0])
        kb.dma("sp", lambda: nc.sync.dma_start(out=ys[NSLOT:NSLOT + 128, :], in_=zt0), reads=[B_zt0], accw=[B_ys])
        vec = ar.alloc(F32, [2, D])
        B_vec = Buf("lnin")
        kb.dma("sp", lambda: nc.sync.dma_start(out=vec, in_=lnin_in.rearrange("a p d -> p a d")), writes=[B_vec])
        xt = [ar.alloc(F32, [D]) for _ in range(2)]
        B_xt = [Buf("xt0"), Buf("xt1")]
        small = [ar.alloc(F32, [40]) for _ in range(2)]
        B_small = [Buf("sm0"), Buf("sm1")]
        for b in (B_R[0], B_R[1]):
            pass
        def load_x(i):
            kb.dma("sp", lambda: nc.sync.dma_start(out=xt[i % 2], in_=x_in[i * 128:(i + 1) * 128, :]), writes=[B_xt[i % 2]])
        load_x(0)
        for i in range(NT):
            s = i % 2
            if i + 1 < NT:
                load_x(i + 1)
            ln_tile(xt[s], B_xt[s], vec[:, 0, :], vec[:, 1, :], B_vec, small[s], B_small[s])
            kb.dma("sp", lambda i=i, s=s: nc.sync.dma_start(out=hres[i * 128:(i + 1) * 128, :], in_=xt[s]), reads=[B_xt[s]], writes=[B_hres[i]])

            def dst(g, i=i):
                k0 = 4 * g
                return R[k0 // 8][:, (k0 % 8):(k0 % 8) + 4, i * 128:(i + 1) * 128]
            transposes_to(xt[s], B_xt[s], dst, [B_R[0], B_R[0], B_R[1], B_R[1]], banks=[0, 1, 2, 3], evac_engs=["act", "dve"])
        kb.barrier()
        ar.release(m)

    def phase_m1(l):
        m = ar.mark()
        bint = ar.alloc(F32, [24])
        convw = ar.alloc(F32, [8, CONV_K])
        cvec = ar.alloc(F32, [5, 8])
        B_sv = Buf("smallvecs")
        kb.dma("sp", lambda: nc.sync.dma_start(out=bint, in_=bin_d[l]), writes=[B_sv])
        kb.dma("sp", lambda: nc.sync.dma_start(out=convw, in_=convw_d[l]), accw=[B_sv])
        kb.dma("sp", lambda: nc.sync.dma_start(out=cvec, in_=cvec_d[l]), accw=[B_sv])
        NWB = 4
        wch = [ar.alloc(BF16, [KD, 128]) for _ in range(NWB)]
        B_wch = [Buf("wch%d" % i) for i in range(NWB)]
        wcount = [0]
        w_in_v = w_in_d[l].rearrange("(k p) c -> p k c", p=128)

        def load_w(col_chunk):
            i = wcount[0] % NWB
            wcount[0] += 1
            kb.dma("pool", lambda: nc.gpsimd.dma_start(out=wch[i], in_=w_in_v[:, :, col_chunk * 128:(col_chunk + 1) * 128]),
                   writes=[B_wch[i]])
            return i

        def proj(wi, bank, t0, n):
            for k in range(KD):
                kb.op("pe", lambda k=k: nc.tensor.matmul(out=PS[bank][:, 0:n], lhsT=wch[wi][:, k, :],
                                                         rhs=hT_chunk((0, 1), k)[:, t0:t0 + n], start=(k == 0), stop=(k == KD - 1)),
                      reads=[B_wch[wi], B_R[0], B_R[1]], writes=[B_PS[bank]] if k == 0 else (),
                      accw=() if k == 0 else [B_PS[bank]], inc=(k == KD - 1))

        pm = ar.mark()
        PADU = 16
        u = ar.alloc(F32, [2, PADU + T])
        sa = ar.alloc(F32, [2, PADU + T])
        sb = ar.alloc(F32, [2, PADU + T])
        dbf = ar.alloc(BF16, [2, T])
        pw = ar.alloc(BF16, [2, 256])
        B_u, B_sa, B_sb, B_dbf, B_pw = Buf("u"), Buf("sa"), Buf("sb"), Buf("dbf"), Buf("pw")
        kb.op("dve", lambda: nc.vector.memset(u[:, :, 0:PADU], 0.0), writes=[B_u])
        kb.op("dve", lambda: nc.vector.memset(sa[:, :, 0:PADU], 0.0), writes=[B_sa])
        kb.op("dve", lambda: nc.vector.memset(sb[:, :, 0:PADU], 0.0), writes=[B_sb])
        pre = [load_w(16), load_w(17)]
        for g in range(4):
            wis = pre
            for cc in range(2):
                for bi, (t0, n) in enumerate(TB):
                    bank = (cc * len(TB) + bi) % 2
                    proj(wis[cc], bank, t0, n)
                    kb.op("act", lambda cc=cc, t0=t0, n=n, bank=bank, g=g: nc.scalar.activation(
                        out=u[:, cc, PADU + t0:PADU + t0 + n], in_=PS[bank][:, 0:n], func=AF.Identity,
                        bias=bint[:, 16 + 2 * g + cc:16 + 2 * g + cc + 1], scale=1.0),
                        reads=[B_PS[bank], B_sv], accw=[B_u])
            if g < 3:
                pre = [load_w(16 + 2 * (g + 1)), load_w(16 + 2 * (g + 1) + 1)]
            kb.dma("pool", lambda g=g: nc.gpsimd.dma_start(out=pw, in_=pool_w_d[l, g].rearrange("(a p) c -> p a c", p=128)),
                   writes=[B_pw])
            kb.op("dve", lambda: nc.vector.tensor_tensor(out=u[:, :, PADU:PADU + 128], in0=u[:, :, PADU:PADU + 128],
                                                         in1=tmask.unsqueeze(1).broadcast_to([128, 2, 128]), op=ALU.mult),
                  reads=[B_const, B_u], writes=[B_u])
            src, B_src = u, B_u
            bufs = [(sa, B_sa), (sb, B_sb)]
            step = 1
            nstep = g + 1
            for si in range(nstep):
                dstb, B_dstb = bufs[si % 2]
                kb.op("dve", lambda src=src, dstb=dstb, step=step: nc.vector.tensor_tensor(
                    out=dstb[:, :, PADU:PADU + T], in0=src[:, :, PADU:PADU + T], in1=src[:, :, PADU - step:PADU - step + T], op=ALU.add),
                    reads=[B_src], accw=[B_dstb])
                src, B_src = dstb, B_dstb
                step *= 2
            wdw = POOL_W[g]
            kb.op("dve", lambda src=src, g=g: nc.vector.tensor_tensor(
                out=src[:, :, PADU:PADU + 128], in0=src[:, :, PADU:PADU + 128],
                in1=invcnt[:, g, :].unsqueeze(1).broadcast_to([128, 2, 128]), op=ALU.mult), reads=[B_const, B_src], writes=[B_src])
            kb.op("dve", lambda src=src: nc.vector.tensor_tensor(
                out=dbf[:, :, 0:128], in0=src[:, :, PADU:PADU + 128], in1=u[:, :, PADU:PADU + 128], op=ALU.subtract),
                reads=[B_src, B_u], writes=[B_dbf])
            kb.op("dve", lambda src=src, wdw=wdw: nc.vector.scalar_tensor_tensor(
                out=dbf[:, :, 128:T], in0=src[:, :, PADU + 128:PADU + T], scalar=1.0 / wdw, in1=u[:, :, PADU + 128:PADU + T],
                op0=ALU.mult, op1=ALU.subtract), reads=[B_src, B_u], accw=[B_dbf])
            for b in range(2):
                for bi, (t0, n) in enumerate(TB):
                    bank = 2 + (b * len(TB) + bi) % 2
                    for a in range(2):
                        kb.op("pe", lambda a=a, b=b, t0=t0, n=n, bank=bank: nc.tensor.matmul(
                            out=PS[bank][:, 0:n], lhsT=pw[:, a, b * 128:(b + 1) * 128], rhs=dbf[:, a, t0:t0 + n],
                            start=(a == 0), stop=(a == 1)), reads=[B_pw, B_dbf],
                            writes=[B_PS[bank]] if a == 0 else (), accw=() if a == 0 else [B_PS[bank]], inc=(a == 1))
                    ch = 2 * g + b
                    kb.op("dve", lambda ch=ch, t0=t0, n=n, bank=bank: nc.vector.tensor_scalar(
                        out=R[2][:, ch, t0:t0 + n], in0=PS[bank][:, 0:n], scalar1=cvec[:, 3, ch:ch + 1], scalar2=cvec[:, 4, ch:ch + 1],
                        op0=ALU.add, op1=ALU.mult), reads=[B_PS[bank], B_sv], accw=[B_R[2]])
        kb.barrier()
        ar.release(pm)
        PADV = 32
        PE_TAPS = 21
        vv = [ar.alloc(F32, [PADV + T]) for _ in range(2)]
        vbf = [ar.alloc(BF16, [PADV + T]) for _ in range(2)]
        dg = [ar.alloc(BF16, [PE_TAPS, 128]) for _ in range(2)]
        sig = [ar.alloc(F32, [512]) for _ in range(2)]
        cacc = [ar.alloc(F32, [T]) for _ in range(2)]
        sq = ar.alloc(F32, [T])
        s1 = ar.alloc(F32, [T])
        s2 = ar.alloc(F32, [T])
        B_vv, B_vbf, B_dg = [Buf("v0"), Buf("v1")], [Buf("vbf0"), Buf("vbf1")], [Buf("dg0"), Buf("dg1")]
        B_sig, B_cacc, B_sq, B_s1, B_s2 = [Buf("sig0"), Buf("sig1")], [Buf("c0"), Buf("c1")], Buf("sq"), Buf("s1"), Buf("s2")
        for q in range(2):
            kb.op("dve", lambda q=q: nc.vector.memset(vv[q][:, 0:PADV], 0.0), writes=[B_vv[q]])
        wsel = {}

        def emit_proj(j):
            wa, wg = wsel[j]
            v, B_v = vv[j % 2], B_vv[j % 2]
            for bi, (t0, n) in enumerate(TB):
                ba, bg = 4 + bi % 2, 6 + bi % 2
                proj(wa, ba, t0, n)
                proj(wg, bg, t0, n)
                sg = sig[bi % 2]
                kb.op("act", lambda sg=sg, bg=bg, n=n: nc.scalar.activation(out=sg[:, 0:n], in_=PS[bg][:, 0:n], func=AF.Sigmoid,
                                                                           bias=bint[:, 8 + j:9 + j], scale=1.0),
                      reads=[B_PS[bg], B_sv], writes=[B_sig[bi % 2]])
                kb.op("dve", lambda sg=sg, ba=ba, t0=t0, n=n: nc.vector.scalar_tensor_tensor(
                    out=v[:, PADV + t0:PADV + t0 + n], in0=PS[ba][:, 0:n], scalar=bint[:, j:j + 1], in1=sg[:, 0:n],
                    op0=ALU.add, op1=ALU.mult), reads=[B_PS[ba], B_sig[bi % 2], B_sv], accw=[B_v])
            kb.op("dve", lambda: nc.vector.tensor_tensor(out=v[:, PADV:PADV + 128], in0=v[:, PADV:PADV + 128], in1=tmask, op=ALU.mult),
                  reads=[B_const, B_v], writes=[B_v])
            kb.op("act", lambda: nc.scalar.copy(out=vbf[j % 2], in_=v), reads=[B_v], writes=[B_vbf[j % 2]])
            kb.op("dve", lambda: nc.vector.tensor_tensor(
                out=dg[j % 2], in0=ident.unsqueeze(1).broadcast_to([128, PE_TAPS, 128]),
                in1=convw[:, j, 0:PE_TAPS].unsqueeze(2).broadcast_to([128, PE_TAPS, 128]), op=ALU.mult),
                reads=[B_const, B_sv], writes=[B_dg[j % 2]])

        def emit_conv(j):
            v, B_v = vv[j % 2], B_vv[j % 2]
            c = cacc[j % 2]
            B_c = B_cacc[j % 2]
            k0 = PE_TAPS
            kb.op("dve", lambda: nc.vector.tensor_scalar(out=c, in0=v[:, 2 + k0:2 + k0 + T], scalar1=convw[:, j, k0:k0 + 1], scalar2=cvec[:, 0, j:j + 1],
                                                         op0=ALU.mult, op1=ALU.add), reads=[B_v, B_sv], writes=[B_c])
            for k in range(k0 + 1, CONV_K):
                kb.op("dve", lambda k=k: nc.vector.scalar_tensor_tensor(
                    out=c, in0=v[:, 2 + k:2 + k + T], scalar=convw[:, j, k:k + 1], in1=c, op0=ALU.mult, op1=ALU.add),
                    reads=[B_v, B_sv], writes=[B_c])
            for bi, (t0, n) in enumerate(TB):
                bank = bi % 4
                for k in range(PE_TAPS):
                    kb.op("pe", lambda k=k, t0=t0, n=n, bank=bank: nc.tensor.matmul(
                        out=PS[bank][:, 0:n], lhsT=dg[j % 2][:, k, :], rhs=vbf[j % 2][:, 2 + k + t0:2 + k + t0 + n],
                        start=(k == 0), stop=(k == PE_TAPS - 1)), reads=[B_dg[j % 2], B_vbf[j % 2]],
                        writes=[B_PS[bank]] if k == 0 else (), accw=() if k == 0 else [B_PS[bank]], inc=(k == PE_TAPS - 1))
                kb.op("dve", lambda t0=t0, n=n, bank=bank: nc.vector.tensor_tensor(out=c[:, t0:t0 + n], in0=c[:, t0:t0 + n], in1=PS[bank][:, 0:n], op=ALU.add),
                      reads=[B_PS[bank], B_c], writes=[B_c])
            kb.op("act", lambda: nc.scalar.activation(out=sq, in_=c, func=AF.Square), reads=[B_c], writes=[B_sq])
            if j == 0:
                kb.op("dve", lambda: nc.vector.tensor_copy(out=s1, in_=c), reads=[B_c], writes=[B_s1])
                kb.op("dve", lambda: nc.vector.tensor_copy(out=s2, in_=sq), reads=[B_sq], writes=[B_s2])
            else:
                kb.op("dve", lambda: nc.vector.tensor_tensor(out=s1, in0=s1, in1=c, op=ALU.add), reads=[B_c, B_s1], writes=[B_s1])
                kb.op("dve", lambda: nc.vector.tensor_tensor(out=s2, in0=s2, in1=sq, op=ALU.add), reads=[B_sq, B_s2], writes=[B_s2])
            kb.dma("sp", lambda: nc.sync.dma_start(out=cT[j], in_=c), reads=[B_c], writes=[B_cT[j]])

        wsel[0] = [load_w(0), load_w(8)]
        emit_proj(0)
        for j in range(8):
            if j + 1 < 8:
                wsel[j + 1] = [load_w(j + 1), load_w(8 + j + 1)]
                emit_proj(j + 1)
            emit_conv(j)
        v = vv[0]
        B_v = B_vv[0]
        meanr = v[:, 0:T]
        rstdr = sq
        for bi, (t0, n) in enumerate(TB):
            b1, b2 = 4 + bi % 2, 6 + bi % 2
            kb.op("pe", lambda b1=b1, t0=t0, n=n: nc.tensor.matmul(out=PS[b1][:, 0:n], lhsT=ones, rhs=s1[:, t0:t0 + n], start=True, stop=True),
                  reads=[B_const, B_s1], writes=[B_PS[b1]])
            kb.op("pe", lambda b2=b2, t0=t0, n=n: nc.tensor.matmul(out=PS[b2][:, 0:n], lhsT=ones, rhs=s2[:, t0:t0 + n], start=True, stop=True),
                  reads=[B_const, B_s2], writes=[B_PS[b2]])
            kb.op("act", lambda b1=b1, t0=t0, n=n: nc.scalar.mul(out=meanr[:, t0:t0 + n], in_=PS[b1][:, 0:n], mul=1.0 / CONV_CH),
                  reads=[B_PS[b1]], accw=[B_v])
            kb.op("dve", lambda t0=t0, n=n: nc.vector.tensor_tensor(out=rstdr[:, t0:t0 + n], in0=meanr[:, t0:t0 + n], in1=meanr[:, t0:t0 + n], op=ALU.mult),
                  reads=[B_v], accw=[B_sq])
            kb.op("dve", lambda b2=b2, t0=t0, n=n: nc.vector.scalar_tensor_tensor(
                out=rstdr[:, t0:t0 + n], in0=PS[b2][:, 0:n], scalar=1.0 / CONV_CH, in1=rstdr[:, t0:t0 + n], op0=ALU.mult, op1=ALU.subtract),
                reads=[B_PS[b2], B_sq], writes=[B_sq])
        kb.op("act", lambda: nc.scalar.activation(out=rstdr, in_=rstdr, func=AF.Ln, bias=EPS, scale=1.0), reads=[B_sq], writes=[B_sq])
        kb.op("act", lambda: nc.scalar.activation(out=rstdr, in_=rstdr, func=AF.Exp, scale=-0.5), reads=[B_sq], writes=[B_sq])
        for j in range(8):
            c = cacc[j % 2]
            B_c = B_cacc[j % 2]
            kb.dma("sp", lambda c=c, j=j: nc.sync.dma_start(out=c, in_=cT[j]), reads=[B_cT[j]], writes=[B_c])
            kb.op("dve", lambda c=c: nc.vector.tensor_tensor(out=c, in0=c, in1=meanr, op=ALU.subtract), reads=[B_v, B_c], writes=[B_c])
            kb.op("dve", lambda c=c: nc.vector.tensor_tensor(out=c, in0=c, in1=rstdr, op=ALU.mult), reads=[B_sq, B_c], writes=[B_c])
            kb.op("act", lambda c=c, j=j: nc.scalar.activation(out=R[0][:, j, :], in_=c, func=AF.Silu, bias=cvec[:, 2, j:j + 1], scale=cvec[:, 1, j:j + 1]),
                  reads=[B_c, B_sv], writes=[B_R[0]] if j == 0 else (), accw=() if j == 0 else [B_R[0]])
        kb.barrier()
        ar.release(m)

    def phase_m2(l):
        m = ar.mark()
        wout = ar.alloc(BF16, [KD, D])
        B_wout = Buf("wout")
        wv = w_out_d[l].rearrange("(k p) c -> p k c", p=128)
        for k in range(KD):
            kb.dma("pool", lambda k=k: nc.gpsimd.dma_start(out=wout[:, k, :], in_=wv[:, k, :]),
                   writes=[B_wout] if k == 0 else (), accw=() if k == 0 else [B_wout])
        vec = Rf[1][:, 0:3 * D].rearrange("p (a b) -> p a b", a=3)
        B_vec = Buf("vecm2")
        kb.dma("sp", lambda: nc.sync.dma_start(out=vec, in_=vecs_in[l, 0:3].rearrange("a p d -> p a d")), writes=[B_vec])
        wr = ar.alloc(F32, [KD, 36])
        brt = ar.alloc(F32, [36])
        B_wr = Buf("wr")
        kb.dma("sp", lambda: nc.sync.dma_start(out=wr, in_=wr_d[l].rearrange("(k p) c -> p k c", p=128)), writes=[B_wr])
        kb.dma("sp", lambda: nc.sync.dma_start(out=brt, in_=br_d[l]), accw=[B_wr])
        ht = [ar.alloc(F32, [D]) for _ in range(2)]
        B_ht = [Buf("ht0"), Buf("ht1")]
        zt = ht
        B_zt = B_ht
        zbf = [ar.alloc(BF16, [D]) for _ in range(2)]
        B_zbf = [Buf("zbf0"), Buf("zbf1")]
        hT32 = Rf[1][:, 3 * D:3 * D + KD * 128].rearrange("p (a b) -> p a b", a=KD)
        B_hT32 = [Buf("hT32_%d" % g) for g in range(4)]
        small = [ar.alloc(F32, [40]) for _ in range(2)]
        B_small = [Buf("sm0"), Buf("sm1")]
        rt = ar.alloc(F32, [512])
        B_rt = Buf("rt")
        kb.op("dve", lambda: nc.vector.memset(base, 0.0), writes=[B_base])
        yreg = (0, 2)

        def load_h(i):
            kb.dma("sp", lambda: nc.sync.dma_start(out=ht[i % 2], in_=hres[i * 128:(i + 1) * 128, :]), reads=[B_hres[i]], writes=[B_ht[i % 2]])
        def emit_main(i):
            for db in range(4):
                for k in range(KD):
                    kb.op("pe", lambda k=k, db=db: nc.tensor.matmul(
                        out=PS[db][:, :], lhsT=hT_chunk(yreg, k)[:, i * 128:(i + 1) * 128], rhs=wout[:, k, db * 512:(db + 1) * 512],
                        start=(k == 0), stop=(k == KD - 1)), reads=[B_R[0], B_R[2], B_wout],
                        writes=[B_PS[db]] if k == 0 else (), accw=() if k == 0 else [B_PS[db]], inc=(k == KD - 1))
        load_h(0)
        emit_main(0)
        for i in range(NT):
            s = i % 2
            if i + 1 < NT:
                load_h(i + 1)
            z = zt[s]
            B_z = B_zt[s]
            for db in range(4):
                kb.op("dve", lambda db=db, z=z, s=s: nc.vector.scalar_tensor_tensor(
                    out=z[:, db * 512:(db + 1) * 512], in0=ht[s][:, db * 512:(db + 1) * 512], scalar=ALPHA, in1=PS[db][:, :],
                    op0=ALU.mult, op1=ALU.add), reads=[B_ht[s], B_PS[db]], writes=[B_z])
            if i + 1 < NT:
                emit_main(i + 1)
            kb.op("dve", lambda z=z: nc.vector.tensor_tensor(out=z, in0=z, in1=vec[:, 0, :], op=ALU.add), reads=[B_vec, B_z], writes=[B_z])
            ln_tile(z, B_z, vec[:, 1, :], vec[:, 2, :], B_vec, small[s], B_small[s])
            kb.dma("sp", lambda i=i, z=z: nc.sync.dma_start(out=hres[i * 128:(i + 1) * 128, :], in_=z), reads=[B_z], writes=[B_hres[i]])
            kb.op("act", lambda z=z, s=s: nc.scalar.copy(out=zbf[s], in_=z), reads=[B_z], writes=[B_zbf[s]])
            transposes_to(z, B_z, lambda g: hT32[:, 4 * g:4 * g + 4, :], B_hT32, banks=[4, 5], evac_engs=["act"])
            for k in range(KD):
                kb.op("pe", lambda k=k: nc.tensor.matmul(out=PS[6][:, 0:36], lhsT=hT32[:, k, :], rhs=wr[:, k, :], start=(k == 0), stop=(k == KD - 1)),
                      reads=[B_hT32[k // 4], B_wr], writes=[B_PS[6]] if k == 0 else (), accw=() if k == 0 else [B_PS[6]], inc=(k == KD - 1))
            routing(i, rt, B_rt, brt, B_wr)
            for kk in range(2):
                kb.dma("pool", lambda i=i, kk=kk, s=s: nc.gpsimd.indirect_dma_start(
                    out=xs, out_offset=bass.IndirectOffsetOnAxis(ap=dest_i[:, i, kk:kk + 1], axis=0), in_=zbf[s], in_offset=None),
                    reads=[B_zbf[s], B_route[i]], accw=[B_xs])
        kb.barrier()
        ar.release(m)

    def routing(i, rt, B_rt, brt, B_wr):
        def V(a, n):
            return rt[:, a:a + n]
        Lg = V(0, 36)
        gmax, gsum, gp = V(36, 1), V(37, 1), V(38, 1)
        gmask = V(40, 4)
        gexp = V(44, 4)
        tmp32 = V(48, 32)
        esel = V(80, 8)
        m1, m2, ng = V(88, 1), V(89, 1), V(90, 1)
        mask1, mask2 = V(96, 8), V(104, 8)
        esel2 = V(112, 8)
        dlt, p1, p2 = V(120, 1), V(121, 1), V(122, 1)
        oh1, oh2, oh = V(128, 32), V(160, 32), V(192, 32)
        slot = V(224, 32)
        valid = V(256, 32)
        tmpb = V(288, 32)

        def dve(fn, w=True):
            kb.op("dve", fn, reads=[B_rt], writes=[B_rt])

        kb.op("dve", lambda: nc.vector.tensor_tensor(out=Lg, in0=PS[6][:, 0:36], in1=brt, op=ALU.add), reads=[B_PS[6], B_wr], writes=[B_rt])
        dve(lambda: nc.vector.tensor_reduce(out=gmax, in_=Lg[:, 0:4], axis=AX.X, op=ALU.max))
        dve(lambda: nc.vector.tensor_scalar(out=gmask, in0=Lg[:, 0:4], scalar1=gmax, scalar2=None, op0=ALU.is_equal))
        dve(lambda: nc.vector.tensor_scalar(out=ng, in0=gmax, scalar1=-1.0, scalar2=None, op0=ALU.mult))
        kb.op("act", lambda: nc.scalar.activation(out=gexp, in_=Lg[:, 0:4], func=AF.Exp, bias=ng, scale=1.0), reads=[B_rt], writes=[B_rt])
        dve(lambda: nc.vector.tensor_reduce(out=gsum, in_=gexp, axis=AX.X, op=ALU.add))
        dve(lambda: nc.vector.reciprocal(out=gp, in_=gsum))
        le = Lg[:, 4:36].rearrange("p (g j) -> p g j", g=4)
        dve(lambda: nc.vector.tensor_tensor(out=tmp32.rearrange("p (g j) -> p g j", g=4), in0=le,
                                            in1=gmask.unsqueeze(2).broadcast_to([128, 4, 8]), op=ALU.mult))
        dve(lambda: nc.vector.tensor_reduce(out=esel, in_=tmp32.rearrange("p (g j) -> p j g", g=4), axis=AX.X, op=ALU.add))
        dve(lambda: nc.vector.tensor_reduce(out=m1, in_=esel, axis=AX.X, op=ALU.max))
        dve(lambda: nc.vector.tensor_scalar(out=mask1, in0=esel, scalar1=m1, scalar2=None, op0=ALU.is_equal))
        dve(lambda: nc.vector.scalar_tensor_tensor(out=esel2, in0=mask1, scalar=-1e30, in1=esel, op0=ALU.mult, op1=ALU.add))
        dve(lambda: nc.vector.tensor_reduce(out=m2, in_=esel2, axis=AX.X, op=ALU.max))
        dve(lambda: nc.vector.tensor_scalar(out=mask2, in0=esel2, scalar1=m2, scalar2=None, op0=ALU.is_equal))
        dve(lambda: nc.vector.tensor_tensor(out=dlt, in0=m2, in1=m1, op=ALU.subtract))
        kb.op("act", lambda: nc.scalar.activation(out=dlt, in_=dlt, func=AF.Exp), reads=[B_rt], writes=[B_rt])
        dve(lambda: nc.vector.tensor_scalar(out=p1, in0=dlt, scalar1=1.0, scalar2=None, op0=ALU.add))
        dve(lambda: nc.vector.reciprocal(out=p1, in_=p1))
        dve(lambda: nc.vector.tensor_tensor(out=p2, in0=dlt, in1=p1, op=ALU.mult))
        kb.op("dve", lambda: nc.vector.tensor_tensor(out=gates[:, i, 0:1], in0=p1, in1=gp, op=ALU.mult), reads=[B_rt], writes=[B_route[i]])
        kb.op("dve", lambda: nc.vector.tensor_tensor(out=gates[:, i, 1:2], in0=p2, in1=gp, op=ALU.mult), reads=[B_rt], accw=[B_route[i]])
        for ohx, mk in ((oh1, mask1), (oh2, mask2)):
            dve(lambda ohx=ohx, mk=mk: nc.vector.tensor_tensor(out=ohx.rearrange("p (g j) -> p g j", g=4),
                                                             in0=gmask.unsqueeze(2).broadcast_to([128, 4, 8]),
                                                             in1=mk.unsqueeze(1).broadcast_to([128, 4, 8]), op=ALU.mult))
        dve(lambda: nc.vector.tensor_tensor(out=oh, in0=oh1, in1=oh2, op=ALU.add))
        if i == 0:
            kb.op("dve", lambda: nc.vector.tensor_scalar(out=oh, in0=oh, scalar1=tokv[:, 0:1], scalar2=None, op0=ALU.mult),
                  reads=[B_rt, B_const], writes=[B_rt])
        kb.op("pe", lambda: nc.tensor.matmul(out=PS[7][:, 0:32], lhsT=tri, rhs=oh, start=True, stop=True), reads=[B_rt, B_const], writes=[B_PS[7]])
        kb.op("pe", lambda: nc.tensor.matmul(out=PS[7][:, 32:64], lhsT=ones, rhs=oh, start=True, stop=True), reads=[B_rt, B_const], accw=[B_PS[7]])
        kb.op("dve", lambda: nc.vector.tensor_tensor(out=slot, in0=PS[7][:, 0:32], in1=base, op=ALU.add), reads=[B_PS[7], B_base, B_rt], writes=[B_rt])
        kb.op("dve", lambda: nc.vector.tensor_tensor(out=base, in0=base, in1=PS[7][:, 32:64], op=ALU.add), reads=[B_PS[7], B_base], writes=[B_base])
        dve(lambda: nc.vector.tensor_scalar(out=valid, in0=slot, scalar1=float(CAP), scalar2=None, op0=ALU.is_lt))
        kb.op("dve", lambda: nc.vector.tensor_tensor(out=slot, in0=slot, in1=ecvec, op=ALU.add), reads=[B_rt, B_const], writes=[B_rt])
        if i == 0:
            kb.op("dve", lambda: nc.vector.tensor_scalar(out=valid, in0=valid, scalar1=tokv[:, 0:1], scalar2=None, op0=ALU.mult),
                  reads=[B_rt, B_const], writes=[B_rt])
        kb.op("dve", lambda: nc.vector.tensor_scalar(out=slot, in0=slot, scalar1=tokv[:, 2:3], scalar2=None, op0=ALU.add),
              reads=[B_rt, B_const], writes=[B_rt])
        dve(lambda: nc.vector.tensor_tensor(out=slot, in0=slot, in1=valid, op=ALU.mult))
        kb.op("dve", lambda: nc.vector.tensor_scalar(out=slot, in0=slot, scalar1=tokv[:, 1:2], scalar2=None, op0=ALU.add),
              reads=[B_rt, B_const], writes=[B_rt])
        for kk, ohx in ((0, oh1), (1, oh2)):
            dve(lambda ohx=ohx: nc.vector.tensor_tensor(out=tmpb, in0=ohx, in1=slot, op=ALU.mult))
            kb.op("dve", lambda kk=kk: nc.vector.tensor_reduce(out=dest_f[:, i, kk:kk + 1], in_=tmpb, axis=AX.X, op=ALU.add),
                  reads=[B_rt], accw=[B_route[i]])
        kb.op("dve", lambda: nc.vector.tensor_copy(out=dest_i[:, i, :], in_=dest_f[:, i, :]), reads=[B_route[i]], accw=[B_route[i]])

    def phase_experts(l):
        m = ar.mark()
        NB = 2
        def rview(i, off_bf, a, b):
            return Rf[i].bitcast(BF16)[:, off_bf:off_bf + a * b].rearrange("p (a b) -> p a b", a=a)
        wg = [rview(i, 0, KD, EH) for i in range(NB)]
        wu = [rview(i, KD * EH, KD, EH) for i in range(NB)]
        wd = [rview(2, i * 4 * D, 4, D) for i in range(NB)]
        xT = [ar.alloc(BF16, [KD, CAP]) for _ in range(NB)]
        B_wg = [Buf("wg%d" % i) for i in range(NB)]
        B_wu = [Buf("wu%d" % i) for i in range(NB)]
        B_wd = [Buf("wd%d" % i) for i in range(NB)]
        B_xT = [Buf("xT%d" % i) for i in range(NB)]
        sgt = [ar.alloc(F32, [CAP]) for _ in range(2)]
        B_sgt = [Buf("sg0"), Buf("sg1")]
        hb = [ar.alloc(BF16, [4, CAP]) for _ in range(2)]
        B_hb = [Buf("hb0"), Buf("hb1")]
        yt = [ar.alloc(F32, [D]) for _ in range(2)]
        B_yt = [Buf("yt0"), Buf("yt1")]

        def load_expert(e):
            s = e % NB
            gv = wg_d[l, e].rearrange("(k p) c -> p k c", p=128)
            uv = wu_d[l, e].rearrange("(k p) c -> p k c", p=128)
            dv = wd_d[l, e].rearrange("(k p) c -> p k c", p=128)
            for q in range(4):
                kb.dma("pool", lambda q=q: nc.gpsimd.dma_start(out=wg[s][:, 4 * q:4 * q + 4, :], in_=gv[:, 4 * q:4 * q + 4, :]),
                       writes=[B_wg[s]] if q == 0 else (), accw=() if q == 0 else [B_wg[s]])
            for q in range(4):
                kb.dma("pool", lambda q=q: nc.gpsimd.dma_start(out=wu[s][:, 4 * q:4 * q + 4, :], in_=uv[:, 4 * q:4 * q + 4, :]),
                       writes=[B_wu[s]] if q == 0 else (), accw=() if q == 0 else [B_wu[s]])
            for q in range(4):
                kb.dma("pool", lambda q=q: nc.gpsimd.dma_start(out=wd[s][:, q, :], in_=dv[:, q, :]),
                       writes=[B_wd[s]] if q == 0 else (), accw=() if q == 0 else [B_wd[s]])
            for k in range(KD):
                kb.dma("sp", lambda k=k: nc.sync.dma_start_transpose(out=xT[s][:, k, :], in_=xs[e * CAP:(e + 1) * CAP, k * 128:(k + 1) * 128]),
                       reads=[B_xs], writes=[B_xT[s]] if k == 0 else (), accw=() if k == 0 else [B_xT[s]])

        load_expert(0)
        for e in range(NEXP):
            s = e % NB
            if e + 1 < NEXP:
                load_expert(e + 1)
            hs = e % 2
            for hc in range(4):
                bg, bu = (hc % 2), 2 + (hc % 2)
                for k in range(KD):
                    kb.op("pe", lambda k=k, hc=hc, bg=bg: nc.tensor.matmul(out=PS[bg][:, 0:CAP], lhsT=wg[s][:, k, hc * 128:(hc + 1) * 128],
                                                                           rhs=xT[s][:, k, :], start=(k == 0), stop=(k == KD - 1)),
                          reads=[B_wg[s], B_xT[s]], writes=[B_PS[bg]] if k == 0 else (), accw=() if k == 0 else [B_PS[bg]], inc=(k == KD - 1))
                for k in range(KD):
                    kb.op("pe", lambda k=k, hc=hc, bu=bu: nc.tensor.matmul(out=PS[bu][:, 0:CAP], lhsT=wu[s][:, k, hc * 128:(hc + 1) * 128],
                                                                           rhs=xT[s][:, k, :], start=(k == 0), stop=(k == KD - 1)),
                          reads=[B_wu[s], B_xT[s]], writes=[B_PS[bu]] if k == 0 else (), accw=() if k == 0 else [B_PS[bu]], inc=(k == KD - 1))
                sg = sgt[hc % 2]
                kb.op("act", lambda sg=sg, bg=bg: nc.scalar.activation(out=sg, in_=PS[bg][:, 0:CAP], func=AF.Silu),
                      reads=[B_PS[bg]], writes=[B_sgt[hc % 2]])
                kb.op("dve", lambda sg=sg, bu=bu, hc=hc, hs=hs: nc.vector.tensor_tensor(out=hb[hs][:, hc, :], in0=sg, in1=PS[bu][:, 0:CAP], op=ALU.mult),
                      reads=[B_sgt[hc % 2], B_PS[bu]], writes=[B_hb[hs]] if hc == 0 else (), accw=() if hc == 0 else [B_hb[hs]])
            for st in range(CAP // 128):
                ys_s = (e * (CAP // 128) + st) % 2
                for db in range(4):
                    bank = 4 + db
                    for hc in range(4):
                        kb.op("pe", lambda hc=hc, db=db, st=st, bank=bank: nc.tensor.matmul(
                            out=PS[bank][:, :], lhsT=hb[hs][:, hc, st * 128:(st + 1) * 128], rhs=wd[s][:, hc, db * 512:(db + 1) * 512],
                            start=(hc == 0), stop=(hc == 3)), reads=[B_hb[hs], B_wd[s]],
                            writes=[B_PS[bank]] if hc == 0 else (), accw=() if hc == 0 else [B_PS[bank]], inc=(hc == 3))
                    eng = "act" if db % 2 == 0 else "dve"
                    if eng == "act":
                        kb.op("act", lambda db=db, bank=bank, ys_s=ys_s: nc.scalar.copy(out=yt[ys_s][:, db * 512:(db + 1) * 512], in_=PS[bank][:, :]),
                              reads=[B_PS[bank]], writes=[B_yt[ys_s]] if db == 0 else (), accw=() if db == 0 else [B_yt[ys_s]])
                    else:
                        kb.op("dve", lambda db=db, bank=bank, ys_s=ys_s: nc.vector.tensor_copy(out=yt[ys_s][:, db * 512:(db + 1) * 512], in_=PS[bank][:, :]),
                              reads=[B_PS[bank]], accw=[B_yt[ys_s]])
                r0 = e * CAP + st * 128
                kb.dma("sp", lambda r0=r0, ys_s=ys_s: nc.sync.dma_start(out=ys[r0:r0 + 128, :], in_=yt[ys_s]), reads=[B_yt[ys_s]], accw=[B_ys])
        kb.barrier()
        ar.release(m)

    def phase_f5(l, last):
        m = ar.mark()
        vec = ar.alloc(F32, [2, D])
        B_vec = Buf("vecf5")
        kb.dma("sp", lambda: nc.sync.dma_start(out=vec, in_=vecs_in[l, 3:5].rearrange("a p d -> p a d")), writes=[B_vec])
        ht = [ar.alloc(F32, [D]) for _ in range(2)]
        B_ht = [Buf("ht0"), Buf("ht1")]
        y1 = [ar.alloc(F32, [D]) for _ in range(2)]
        y2 = [ar.alloc(F32, [D]) for _ in range(2)]
        B_y1 = [Buf("y1_0"), Buf("y1_1")]
        B_y2 = [Buf("y2_0"), Buf("y2_1")]
        small = [ar.alloc(F32, [40]) for _ in range(2)]
        B_small = [Buf("sm0"), Buf("sm1")]
        def load_t(i):
            s = i % 2
            kb.dma("sp", lambda: nc.sync.dma_start(out=ht[s], in_=hres[i * 128:(i + 1) * 128, :]), reads=[B_hres[i]], writes=[B_ht[s]])
            kb.dma("pool", lambda: nc.gpsimd.indirect_dma_start(
                out=y1[s], out_offset=None, in_=ys, in_offset=bass.IndirectOffsetOnAxis(ap=dest_i[:, i, 0:1], axis=0)),
                reads=[B_ys, B_route[i]], writes=[B_y1[s]])
            kb.dma("pool", lambda: nc.gpsimd.indirect_dma_start(
                out=y2[s], out_offset=None, in_=ys, in_offset=bass.IndirectOffsetOnAxis(ap=dest_i[:, i, 1:2], axis=0)),
                reads=[B_ys, B_route[i]], writes=[B_y2[s]])
        load_t(0)
        for i in range(NT):
            s = i % 2
            if i + 1 < NT:
                load_t(i + 1)
            z = ht[s]
            B_z = B_ht[s]
            kb.op("dve", lambda z=z, s=s, i=i: nc.vector.tensor_scalar(out=y1[s], in0=y1[s], scalar1=gates[:, i, 0:1], scalar2=None, op0=ALU.mult),
                  reads=[B_route[i], B_y1[s]], writes=[B_y1[s]])
            kb.op("dve", lambda z=z, s=s: nc.vector.scalar_tensor_tensor(out=z, in0=z, scalar=ALPHA, in1=y1[s], op0=ALU.mult, op1=ALU.add),
                  reads=[B_y1[s], B_z], writes=[B_z])
            kb.op("dve", lambda z=z, s=s, i=i: nc.vector.scalar_tensor_tensor(out=z, in0=y2[s], scalar=gates[:, i, 1:2], in1=z, op0=ALU.mult, op1=ALU.add),
                  reads=[B_y2[s], B_route[i], B_z], writes=[B_z])
            ln_tile(z, B_z, vec[:, 0, :], vec[:, 1, :], B_vec, small[s], B_small[s])
            if last:
                if i >= 1:
                    kb.dma("sp", lambda i=i, z=z: nc.sync.dma_start(out=out_d[(i - 1) * 128:i * 128, :], in_=z), reads=[B_z], accw=[B_out])
            else:
                kb.dma("sp", lambda i=i, z=z: nc.sync.dma_start(out=hres[i * 128:(i + 1) * 128, :], in_=z), reads=[B_z], writes=[B_hres[i]])

                def dst(g, i=i):
                    k0 = 4 * g
                    return R[k0 // 8][:, (k0 % 8):(k0 % 8) + 4, i * 128:(i + 1) * 128]
                transposes_to(z, B_z, dst, [B_R[0], B_R[0], B_R[1], B_R[1]], banks=[0, 1, 2, 3], evac_engs=["act", "dve"])
            if debug and last is False and False:
                pass
        kb.barrier()
        ar.release(m)

    B_out = Buf("out")
    stages = []
    phase_input_ln()
    if stop_after == "exp_dma":
        mm = ar.mark()
        wchx = [ar.alloc(BF16, [KD, 128]) for _ in range(4)]
        B_wx = [Buf("wx%d" % i) for i in range(4)]
        wv_x = w_in_d[0].rearrange("(k p) c -> p k c", p=128)
        for cc in range(24):
            kb.dma("pool", lambda cc=cc: nc.gpsimd.dma_start(out=wchx[cc % 4], in_=wv_x[:, :, cc * 128:(cc + 1) * 128]), writes=[B_wx[cc % 4]])
            import os
            if os.environ.get("EXP_SERIAL"):
                kb.wait_all("pool", B_wx)
        kb.barrier()
        ar.release(mm)
    done = (stop_after in ("ln0", "exp_dma"))
    if done and debug:
        for ri in range(3):
            kb.dma("sp", lambda ri=ri: nc.sync.dma_start(out=dbg_y[ri], in_=R[ri]), reads=[B_R[ri]], writes=[B_out])
    for l in range(depth):
        if done:
            break
        phase_m1(l)
        if stop_after == "m1_%d" % l:
            for ri in range(3):
                kb.dma("sp", lambda ri=ri: nc.sync.dma_start(out=dbg_y[ri], in_=R[ri]), reads=[B_R[ri]], writes=[B_out])
            break
        phase_m2(l)
        if stop_after == "m2_%d" % l:
            break
        if debug:
            kb.dma("sp", lambda: nc.sync.dma_start(out=dbg_r[0], in_=dest_f), reads=B_route, writes=[B_out])
            kb.dma("sp", lambda: nc.sync.dma_start(out=dbg_r[1], in_=gates), reads=B_route, writes=[B_out])
        phase_experts(l)
        if stop_after == "ex_%d" % l:
            break
        last = (l == depth - 1) and not debug
        phase_f5(l, last)
        if stop_after == "f5_%d" % l:
            break
    kb.wait_all("sp", [B_out, B_xs, B_ys] + B_cT + B_hres)
    kb.wait_all("sp", B_R + B_PS)
    kb.close()
    for cm in reversed(ps_cms):
        cm.__exit__(None, None, None)
    arena_cm.__exit__(None, None, None)
    return nc


def _rep(v):
    return np.ascontiguousarray(np.broadcast_to(np.asarray(v, np.float32)[None, :], (128, v.shape[-1])))


def prepare_inputs(inp):
    f = lambda a: np.ascontiguousarray(np.asarray(a, dtype=np.float32))
    x = f(inp["x"])[0]
    meta = f(inp["meta_tokens"])
    stream = np.concatenate([np.zeros((HALO - NMETA, D), np.float32), meta, x], axis=0)
    L = DEPTH
    lnin = np.stack([_rep(f(inp["ln_in_g"])), _rep(f(inp["ln_in_b"]))])
    vecs = np.stack([np.stack([_rep(f(inp[k])[l]) for k in ("b_out", "ln_mix_g", "ln_mix_b", "ln_ffn_g", "ln_ffn_b")]) for l in range(L)])
    b_in_t = np.ascontiguousarray(f(inp["b_in"]).reshape(L, 24, 128).transpose(0, 2, 1))
    conv_w_t = np.ascontiguousarray(f(inp["conv_w"])[:, :, 0, :].reshape(L, CONV_K, 8, 128).transpose(0, 3, 2, 1))
    cv = [f(inp["conv_b"]), f(inp["conv_ln_g"]), f(inp["conv_ln_b"]), f(inp["pool_b"]).reshape(L, 1024), f(inp["pool_scale"])]
    cvec = np.ascontiguousarray(np.stack([c.reshape(L, 8, 128).transpose(0, 2, 1) for c in cv], axis=2))
    wr = np.ascontiguousarray(np.concatenate([f(inp["router_group_w"]), f(inp["router_expert_w"])], axis=2))
    brv = np.concatenate([f(inp["router_group_b"]), f(inp["router_expert_b"])], axis=1)
    br = np.ascontiguousarray(np.broadcast_to(brv[:, None, :], (L, 128, 36)))
    ident = np.eye(128, dtype=np.float32)
    tri = np.triu(np.ones((128, 128), np.float32), 1)
    ones = np.ones((128, 128), np.float32)
    consts = np.ascontiguousarray(np.stack([ident, tri, ones], axis=1))
    ecvec = np.ascontiguousarray(np.broadcast_to((np.arange(NEXP, dtype=np.float32) * CAP)[None, :], (128, NEXP)))
    shared = dict(consts=consts, ecvec=ecvec, lnin=lnin, vecs=vecs, w_in=f(inp["w_in"]), w_out=f(inp["w_out"]),
                  pool_w=f(inp["pool_w"]), b_in_t=b_in_t, conv_w_t=conv_w_t, cvec=cvec, wr=wr, br=br,
                  w_gate=f(inp["w_gate"]), w_up=f(inp["w_up"]), w_down=f(inp["w_down"]))
    maps = []
    for c in range(NCORES):
        r0 = c * TOK_OUT
        xc = np.ascontiguousarray(stream[r0:r0 + T])
        tm = np.ones((128,), np.float32)
        ic = np.stack([np.full((128,), 1.0 / w, np.float32) for w in POOL_W])
        if c == 0:
            tm[:HALO - NMETA] = 0.0
            p = np.arange(128) - (HALO - NMETA)
            for gi, w in enumerate(POOL_W):
                ic[gi] = np.where(p >= 0, 1.0 / np.minimum(w, np.maximum(p, 0) + 1), 1.0 / w).astype(np.float32)
        d = dict(shared)
        d["x"] = xc
        d["tmask"] = np.ascontiguousarray(np.broadcast_to(tm[None, :], (128, 128)))
        d["invcnt"] = np.ascontiguousarray(np.broadcast_to(ic[None, :, :], (128, 4, 128)))
        tv = np.ones((128, 4), np.float32)
        tv[:, 0] = tm
        tv[:, 1] = TRASH + np.arange(128)
        tv[:, 2] = -(TRASH + np.arange(128))
        d["tokv"] = tv
        maps.append(d)
    return maps


_NC_CACHE = {}


def kernel(**inputs):
    maps = prepare_inputs(inputs)
    if "nc" not in _NC_CACHE:
        _NC_CACHE["nc"] = build_program()
    nc = _NC_CACHE["nc"]
    res = run_bass_kernel_spmd(nc, maps, core_ids=list(range(NCORES)))
    out = np.concatenate([np.asarray(r["out"], dtype=np.float32) for r in res.results], axis=0)
    return out.reshape(1, SEQ, D)
```
